# Optimizing a Trainium2 kernel written in Bass

```python
import math
import jax, jax.numpy as jnp
from jax import lax
import numpy as np

D_MODEL = 1024
BATCH = 16
SEQ = 2048
DEPTH = 2

PLE_DIM = 256
N_EVEN = (DEPTH + 1) // 2
N_ODD = DEPTH // 2
NORM_EPS = 1e-6
CONV_WIDTH = 4

FOX_HEAD_DIM = 64
FOX_HEADS = (D_MODEL // 2) // FOX_HEAD_DIM
FOX_W = FOX_HEADS * FOX_HEAD_DIM
FOX_Q_BLOCK = 128
MLSTM_HEAD_DIM = 128
MLSTM_HEADS = (D_MODEL // 2) // MLSTM_HEAD_DIM
MLSTM_W = MLSTM_HEADS * MLSTM_HEAD_DIM
MLSTM_CHUNK = 64
MOBA_HEAD_DIM = 64
MOBA_HEADS = (D_MODEL // 2) // MOBA_HEAD_DIM
MOBA_W = MOBA_HEADS * MOBA_HEAD_DIM
MOBA_BLOCK = 256
MOBA_TOPK = 3
MOBA_Q_CHUNK = 16
SSD_HEAD_DIM = 64
SSD_HEADS = (D_MODEL // 2) // SSD_HEAD_DIM
SSD_W = SSD_HEADS * SSD_HEAD_DIM
SSD_GROUPS = 2
SSD_STATE = 128
SSD_BC = SSD_GROUPS * SSD_STATE
SSD_CONV_CH = SSD_W + 2 * SSD_BC
SSD_CHUNK = 64
MOE_GROUPS = 4
MOE_EXPERTS_PER_GROUP = 4
MOE_EXPERTS = MOE_GROUPS * MOE_EXPERTS_PER_GROUP
MOE_TOPK = 2
MOE_HIDDEN = D_MODEL // 4

EVEN_SIZES = (FOX_W, FOX_W, FOX_W, FOX_HEADS, FOX_W,
              MLSTM_W, MLSTM_W, MLSTM_W, MLSTM_HEADS, MLSTM_HEADS, MLSTM_W)
EVEN_COLS = sum(EVEN_SIZES)
EVEN_MIX = FOX_W + MLSTM_W
ODD_SIZES = (MOBA_W, MOBA_W, MOBA_W, SSD_W, SSD_W, SSD_BC, SSD_BC, SSD_HEADS)
ODD_COLS = sum(ODD_SIZES)
ODD_MIX = MOBA_W + SSD_W

kernel_name = 'hybrid_fox_mlstm_moba_ssd_hmoe'


def rms_norm(x, g, eps=NORM_EPS):
    xf = x.astype(jnp.float32)
    y = xf * lax.rsqrt(jnp.mean(xf * xf, axis=-1, keepdims=True) + eps)
    return (y * g.astype(jnp.float32)).astype(x.dtype)


def split_cols(u, sizes):
    offs = np.cumsum(np.array(sizes))[:-1].tolist()
    return jnp.split(u, offs, axis=-1)


def split_heads(y, n_heads):
    bsz, seq, width = y.shape
    return y.reshape(bsz, seq, n_heads, width // n_heads).transpose(0, 2, 1, 3)


def merge_heads(y):
    bsz, n_heads, seq, hd = y.shape
    return y.transpose(0, 2, 1, 3).reshape(bsz, seq, n_heads * hd)


def causal_dwconv(x, w, b):
    width = w.shape[0]
    seq = x.shape[1]
    xp = jnp.pad(x, ((0, 0), (width - 1, 0), (0, 0)))
    y = b
    for j in range(width):
        y = y + xp[:, j:j + seq] * w[j]
    return y


def forgetting_attention(q, k, v, log_f):
    bsz, n_heads, seq, hd = q.shape
    cum_f = jnp.cumsum(log_f, axis=-1)
    scale = hd ** -0.5
    kpos = jnp.arange(seq)

    def q_block(bi):
        start = bi * FOX_Q_BLOCK
        qb = lax.dynamic_slice_in_dim(q, start, FOX_Q_BLOCK, axis=2)
        fb = lax.dynamic_slice_in_dim(cum_f, start, FOX_Q_BLOCK, axis=2)
        qpos = start + jnp.arange(FOX_Q_BLOCK)
        logits = jnp.einsum('bhqd,bhkd->bhqk', qb, k).astype(jnp.float32) * scale
        logits = logits + fb[..., :, None] - cum_f[..., None, :]
        logits = jnp.where(kpos[None, :] <= qpos[:, None], logits, -jnp.inf)
        probs = jax.nn.softmax(logits, axis=-1).astype(v.dtype)
        return jnp.einsum('bhqk,bhkd->bhqd', probs, v)

    out = lax.map(q_block, jnp.arange(seq // FOX_Q_BLOCK))
    return out.transpose(1, 2, 0, 3, 4).reshape(bsz, n_heads, seq, hd)


def mlstm_chunkwise(q, k, v, log_i, log_f):
    bsz, n_heads, seq, hd = q.shape
    L = MLSTM_CHUNK
    nc = seq // L
    f32 = jnp.float32
    q = q.astype(f32)
    k = k.astype(f32) * (hd ** -0.5)
    v = v.astype(f32)

    def chunks(a):
        a = a.reshape(a.shape[:2] + (nc, L) + a.shape[3:])
        return jnp.moveaxis(a, 2, 0)

    tri = jnp.tril(jnp.ones((L, L), dtype=bool))

    def step(carry, inp):
        c_st, n_st, m_st = carry
        qc, kc, vc, ic, fc = inp
        b = jnp.cumsum(fc, axis=-1)
        log_d = b[..., :, None] - b[..., None, :] + ic[..., None, :]
        log_d = jnp.where(tri, log_d, -jnp.inf)
        m_inter = b + m_st[..., None]
        m_t = jnp.maximum(m_inter, jnp.max(log_d, axis=-1))
        d_mat = jnp.exp(log_d - m_t[..., None])
        inter = jnp.exp(m_inter - m_t)
        w_qk = jnp.einsum('bhld,bhsd->bhls', qc, kc) * d_mat
        num = (jnp.einsum('bhls,bhsd->bhld', w_qk, vc)
               + inter[..., None] * jnp.einsum('bhld,bhde->bhle', qc, c_st))
        den = w_qk.sum(axis=-1) + inter * jnp.einsum('bhld,bhd->bhl', qc, n_st)
        h = num / jnp.maximum(jnp.abs(den), jnp.exp(-m_t))[..., None]
        b_last = b[..., -1]
        log_w = b_last[..., None] - b + ic
        m_new = jnp.maximum(b_last + m_st, jnp.max(log_w, axis=-1))
        w = jnp.exp(log_w - m_new[..., None])
        decay = jnp.exp(b_last + m_st - m_new)
        c_new = decay[..., None, None] * c_st + jnp.einsum('bhl,bhld,bhle->bhde', w, kc, vc)
        n_new = decay[..., None] * n_st + jnp.einsum('bhl,bhld->bhd', w, kc)
        return (c_new, n_new, m_new), h

    init = (jnp.zeros((bsz, n_heads, hd, hd), f32),
            jnp.zeros((bsz, n_heads, hd), f32),
            jnp.zeros((bsz, n_heads), f32))
    _, hs = lax.scan(step, init, (chunks(q), chunks(k), chunks(v),
                                  chunks(log_i.astype(f32)), chunks(log_f.astype(f32))))
    return jnp.moveaxis(hs, 0, 2).reshape(bsz, n_heads, seq, hd)


def moba_attention(q, k, v):
    bsz, n_heads, seq, hd = q.shape
    nb = -(-seq // MOBA_BLOCK)
    seq_p = nb * MOBA_BLOCK
    pad = ((0, 0), (0, 0), (0, seq_p - seq), (0, 0))
    qp, kp, vp = jnp.pad(q, pad), jnp.pad(k, pad), jnp.pad(v, pad)
    kb = kp.reshape(bsz, n_heads, nb, MOBA_BLOCK, hd)
    vb = vp.reshape(bsz, n_heads, nb, MOBA_BLOCK, hd)
    k_mean = jnp.mean(kb.astype(jnp.float32), axis=3)
    k_sel = min(MOBA_TOPK, nb)
    scale = hd ** -0.5
    bi = jnp.arange(bsz)[:, None, None, None]
    hi = jnp.arange(n_heads)[None, :, None, None]

    def q_chunk(ci):
        start = ci * MOBA_Q_CHUNK
        qc = lax.dynamic_slice_in_dim(qp, start, MOBA_Q_CHUNK, axis=2)
        qpos = start + jnp.arange(MOBA_Q_CHUNK)
        own = start // MOBA_BLOCK
        gate = jnp.einsum('bhqd,bhnd->bhqn', qc.astype(jnp.float32), k_mean)
        gate = jnp.where(jnp.arange(nb) < own, gate, -jnp.inf)
        g_val, g_idx = lax.top_k(gate, k_sel)
        valid = jnp.isfinite(g_val)
        k_g = kb[bi, hi, g_idx].reshape(bsz, n_heads, MOBA_Q_CHUNK, k_sel * MOBA_BLOCK, hd)
        v_g = vb[bi, hi, g_idx].reshape(bsz, n_heads, MOBA_Q_CHUNK, k_sel * MOBA_BLOCK, hd)
        logit_past = jnp.einsum('bhqd,bhqnd->bhqn', qc, k_g).astype(jnp.float32) * scale
        logit_past = jnp.where(jnp.repeat(valid, MOBA_BLOCK, axis=-1), logit_past, -jnp.inf)
        k_own = lax.dynamic_slice_in_dim(kp, own * MOBA_BLOCK, MOBA_BLOCK, axis=2)
        v_own = lax.dynamic_slice_in_dim(vp, own * MOBA_BLOCK, MOBA_BLOCK, axis=2)
        kpos = own * MOBA_BLOCK + jnp.arange(MOBA_BLOCK)
        logit_own = jnp.einsum('bhqd,bhkd->bhqk', qc, k_own).astype(jnp.float32) * scale
        logit_own = jnp.where(kpos[None, :] <= qpos[:, None], logit_own, -jnp.inf)
        probs = jax.nn.softmax(jnp.concatenate([logit_past, logit_own], axis=-1), axis=-1)
        probs = probs.astype(v.dtype)
        n_past = k_sel * MOBA_BLOCK
        return (jnp.einsum('bhqn,bhqnd->bhqd', probs[..., :n_past], v_g)
                + jnp.einsum('bhqk,bhkd->bhqd', probs[..., n_past:], v_own))

    out = lax.map(q_chunk, jnp.arange(seq_p // MOBA_Q_CHUNK))
    out = out.transpose(1, 2, 0, 3, 4).reshape(bsz, n_heads, seq_p, hd)
    return out[:, :, :seq]


def segsum(a):
    L = a.shape[-1]
    cs = jnp.cumsum(a, axis=-1)
    out = cs[..., :, None] - cs[..., None, :]
    return jnp.where(jnp.tril(jnp.ones((L, L), dtype=bool)), out, -jnp.inf)


def ssd_chunked(x, dt, a, b_in, c_in):
    bsz, seq, n_heads, hp = x.shape
    ns = b_in.shape[-1]
    L = SSD_CHUNK
    nc = seq // L
    f32 = jnp.float32
    xdt = (x.astype(f32) * dt[..., None]).reshape(bsz, nc, L, n_heads, hp)
    a_dt = (a * dt).reshape(bsz, nc, L, n_heads).transpose(0, 1, 3, 2)
    bc = b_in.astype(f32).reshape(bsz, nc, L, n_heads, ns)
    cc = c_in.astype(f32).reshape(bsz, nc, L, n_heads, ns)
    a_cum = jnp.cumsum(a_dt, axis=-1)
    l_mat = jnp.exp(segsum(a_dt))
    cb = jnp.einsum('bclhn,bcshn->bchls', cc, bc)
    y_diag = jnp.einsum('bchls,bcshp->bclhp', cb * l_mat, xdt)
    decay_states = jnp.exp(a_cum[..., -1:] - a_cum)
    states = jnp.einsum('bclhn,bchl,bclhp->bchpn', bc, decay_states, xdt)
    chunk_decay = jnp.exp(a_cum[..., -1])

    def step(h, inp):
        st, dec = inp
        return dec[..., None, None] * h + st, h

    h0 = jnp.zeros((bsz, n_heads, hp, ns), f32)
    _, prev = lax.scan(step, h0, (jnp.moveaxis(states, 1, 0), jnp.moveaxis(chunk_decay, 1, 0)))
    prev = jnp.moveaxis(prev, 0, 1)
    y_off = jnp.einsum('bclhn,bchpn,bchl->bclhp', cc, prev, jnp.exp(a_cum))
    return (y_diag + y_off).reshape(bsz, seq, n_heads, hp)


def even_mixers(h, w_in, fox_b_f, fox_qn_g, fox_kn_g, conv_w, conv_b, b_i, b_f, norm_g):
    f32 = jnp.float32
    u = jnp.einsum('bsd,de->bse', h, w_in)
    fq, fk, fv, ff, fo, mq, mk, mv, mi, mf, mo = split_cols(u, EVEN_SIZES)
    q = rms_norm(split_heads(fq, FOX_HEADS), fox_qn_g)
    k = rms_norm(split_heads(fk, FOX_HEADS), fox_kn_g)
    v = split_heads(fv, FOX_HEADS)
    log_f = jax.nn.log_sigmoid(ff.astype(f32) + fox_b_f).transpose(0, 2, 1)
    out_a = merge_heads(forgetting_attention(q, k, v, log_f)) * jax.nn.sigmoid(fo)
    qk = jax.nn.silu(causal_dwconv(jnp.concatenate([mq, mk], axis=-1), conv_w, conv_b))
    mq, mk = jnp.split(qk, 2, axis=-1)
    log_i = (mi.astype(f32) + b_i).transpose(0, 2, 1)
    log_fm = jax.nn.log_sigmoid(mf.astype(f32) + b_f).transpose(0, 2, 1)
    hc = mlstm_chunkwise(split_heads(mq, MLSTM_HEADS), split_heads(mk, MLSTM_HEADS),
                         split_heads(mv, MLSTM_HEADS), log_i, log_fm)
    hc = rms_norm(hc, norm_g.reshape(MLSTM_HEADS, 1, MLSTM_HEAD_DIM)).astype(h.dtype)
    out_b = merge_heads(hc) * jax.nn.sigmoid(mo)
    return jnp.concatenate([out_a, out_b], axis=-1)


def odd_mixers(h, w_in, moba_qn_g, moba_kn_g, conv_w, conv_b, dt_bias, a_log, d_skip, norm_g):
    bsz, seq, _ = h.shape
    u = jnp.einsum('bsd,de->bse', h, w_in)
    cq, ck, cv, z, xs, bs, cs, dts = split_cols(u, ODD_SIZES)
    q = rms_norm(split_heads(cq, MOBA_HEADS), moba_qn_g)
    k = rms_norm(split_heads(ck, MOBA_HEADS), moba_kn_g)
    v = split_heads(cv, MOBA_HEADS)
    out_c = merge_heads(moba_attention(q, k, v))
    xbc = jax.nn.silu(causal_dwconv(jnp.concatenate([xs, bs, cs], axis=-1), conv_w, conv_b))
    xs, bs, cs = split_cols(xbc, (SSD_W, SSD_BC, SSD_BC))
    dt = jax.nn.softplus(dts.astype(jnp.float32) + dt_bias)
    a = -jnp.exp(a_log.astype(jnp.float32))
    xh = xs.reshape(bsz, seq, SSD_HEADS, SSD_HEAD_DIM)
    rep = SSD_HEADS // SSD_GROUPS
    bh = jnp.repeat(bs.reshape(bsz, seq, SSD_GROUPS, SSD_STATE), rep, axis=2)
    ch = jnp.repeat(cs.reshape(bsz, seq, SSD_GROUPS, SSD_STATE), rep, axis=2)
    y = ssd_chunked(xh, dt, a, bh, ch) + d_skip[:, None] * xh
    y = y.reshape(bsz, seq, SSD_W) * jax.nn.silu(z)
    y = rms_norm(y.reshape(bsz, seq, SSD_GROUPS, SSD_W // SSD_GROUPS),
                 norm_g.reshape(SSD_GROUPS, SSD_W // SSD_GROUPS))
    out_d = y.reshape(bsz, seq, SSD_W).astype(h.dtype)
    return jnp.concatenate([out_c, out_d], axis=-1)


def hier_moe(h, w_group, b_group, w_router, b_router, w_gate, w_up, w_down):
    bsz, seq, dm = h.shape
    t = h.reshape(-1, dm)
    g_logits = jnp.einsum('td,dg->tg', t, w_group).astype(jnp.float32) + b_group
    g_prob = jax.nn.softmax(g_logits, axis=-1)
    g_idx = jnp.argmax(g_logits, axis=-1)
    g_w = jnp.take_along_axis(g_prob, g_idx[:, None], axis=1)[:, 0]
    e_logits = (jnp.einsum('td,de->te', t, w_router).astype(jnp.float32) + b_router)
    e_logits = e_logits.reshape(-1, MOE_GROUPS, MOE_EXPERTS_PER_GROUP)
    e_in = jnp.take_along_axis(e_logits, g_idx[:, None, None], axis=1)[:, 0]
    top_v, top_i = lax.top_k(e_in, MOE_TOPK)
    top_w = jax.nn.softmax(top_v, axis=-1)
    w_in_group = jnp.sum(top_w[..., None] * jax.nn.one_hot(top_i, MOE_EXPERTS_PER_GROUP), axis=1)
    combine = (jax.nn.one_hot(g_idx, MOE_GROUPS)[:, :, None]
               * (g_w[:, None] * w_in_group)[:, None, :]).astype(h.dtype)
    out = jnp.zeros_like(t)
    for g in range(MOE_GROUPS):
        a = jnp.einsum('td,edf->tef', t, w_gate[g])
        b = jnp.einsum('td,edf->tef', t, w_up[g])
        hid = jax.nn.silu(a) * b * combine[:, g, :, None]
        out = out + jnp.einsum('tef,efd->td', hid, w_down[g])
    return out.reshape(bsz, seq, dm)


def per_layer_embedding(x, p_i, w_proj, w_gate, gate_norm_g, out_norm_g):
    e = jnp.einsum('bsk,kd->bsd', p_i, w_proj)
    gate = jax.nn.sigmoid(jnp.einsum('bsd,de->bse', rms_norm(x, gate_norm_g), w_gate))
    return rms_norm(e * gate, out_norm_g)


def setup_inputs(seed: int = 0) -> dict:
    key = jax.random.key(seed)
    k = jax.random.split(key, 35)
    f32 = jnp.float32

    def nrm(kk, shape, s):
        return s * jax.random.normal(kk, shape, f32)

    def gain(kk, shape):
        return 1.0 + 0.05 * jax.random.normal(kk, shape, f32)

    dt_u = jax.random.uniform(k[19], (N_ODD, SSD_HEADS), f32)
    dt0 = jnp.exp(dt_u * (math.log(0.1) - math.log(1e-3)) + math.log(1e-3))
    return {
        'x': nrm(k[0], (BATCH, SEQ, D_MODEL), 1.0),
        'p': nrm(k[1], (DEPTH, BATCH, SEQ, PLE_DIM), 1.0),
        'norm1_g': gain(k[2], (DEPTH, D_MODEL)),
        'norm2_g': gain(k[3], (DEPTH, D_MODEL)),
        'ev_w_in': nrm(k[4], (N_EVEN, D_MODEL, EVEN_COLS), D_MODEL ** -0.5),
        'ev_fox_b_f': 2.0 + nrm(k[5], (N_EVEN, FOX_HEADS), 0.5),
        'ev_fox_qn_g': gain(k[6], (N_EVEN, FOX_HEAD_DIM)),
        'ev_fox_kn_g': gain(k[7], (N_EVEN, FOX_HEAD_DIM)),
        'ev_mlstm_conv_w': nrm(k[8], (N_EVEN, CONV_WIDTH, 2 * MLSTM_W), CONV_WIDTH ** -0.5),
        'ev_mlstm_conv_b': nrm(k[9], (N_EVEN, 2 * MLSTM_W), 0.01),
        'ev_mlstm_b_i': nrm(k[10], (N_EVEN, MLSTM_HEADS), 0.1),
        'ev_mlstm_b_f': jnp.linspace(3.0, 6.0, MLSTM_HEADS, dtype=f32)[None, :]
                        + nrm(k[11], (N_EVEN, MLSTM_HEADS), 0.1),
        'ev_mlstm_norm_g': gain(k[12], (N_EVEN, MLSTM_W)),
        'ev_w_out': nrm(k[13], (N_EVEN, EVEN_MIX, D_MODEL), EVEN_MIX ** -0.5),
        'od_w_in': nrm(k[14], (N_ODD, D_MODEL, ODD_COLS), D_MODEL ** -0.5),
        'od_moba_qn_g': gain(k[15], (N_ODD, MOBA_HEAD_DIM)),
        'od_moba_kn_g': gain(k[16], (N_ODD, MOBA_HEAD_DIM)),
        'od_ssd_conv_w': nrm(k[17], (N_ODD, CONV_WIDTH, SSD_CONV_CH), CONV_WIDTH ** -0.5),
        'od_ssd_conv_b': nrm(k[18], (N_ODD, SSD_CONV_CH), 0.01),
        'od_ssd_dt_bias': dt0 + jnp.log(-jnp.expm1(-dt0)),
        'od_ssd_A_log': jnp.log(jax.random.uniform(k[20], (N_ODD, SSD_HEADS), f32, 1.0, 16.0)),
        'od_ssd_D': 1.0 + nrm(k[21], (N_ODD, SSD_HEADS), 0.1),
        'od_ssd_norm_g': gain(k[22], (N_ODD, SSD_W)),
        'od_w_out': nrm(k[23], (N_ODD, ODD_MIX, D_MODEL), ODD_MIX ** -0.5),
        'moe_w_group': nrm(k[24], (DEPTH, D_MODEL, MOE_GROUPS), D_MODEL ** -0.5),
        'moe_b_group': nrm(k[25], (DEPTH, MOE_GROUPS), 0.01),
        'moe_w_router': nrm(k[26], (DEPTH, D_MODEL, MOE_EXPERTS), D_MODEL ** -0.5),
        'moe_b_router': nrm(k[27], (DEPTH, MOE_EXPERTS), 0.01),
        'moe_w_gate': nrm(k[28], (DEPTH, MOE_GROUPS, MOE_EXPERTS_PER_GROUP, D_MODEL, MOE_HIDDEN), D_MODEL ** -0.5),
        'moe_w_up': nrm(k[29], (DEPTH, MOE_GROUPS, MOE_EXPERTS_PER_GROUP, D_MODEL, MOE_HIDDEN), D_MODEL ** -0.5),
        'moe_w_down': nrm(k[30], (DEPTH, MOE_GROUPS, MOE_EXPERTS_PER_GROUP, MOE_HIDDEN, D_MODEL), MOE_HIDDEN ** -0.5),
        'ple_w_proj': nrm(k[31], (DEPTH, PLE_DIM, D_MODEL), PLE_DIM ** -0.5),
        'ple_w_gate': nrm(k[32], (DEPTH, D_MODEL, D_MODEL), D_MODEL ** -0.5),
        'ple_gate_norm_g': gain(k[33], (DEPTH, D_MODEL)),
        'ple_out_norm_g': gain(k[34], (DEPTH, D_MODEL)),
    }


def reference(x, p, norm1_g, norm2_g, ev_w_in, ev_fox_b_f, ev_fox_qn_g, ev_fox_kn_g,
              ev_mlstm_conv_w, ev_mlstm_conv_b, ev_mlstm_b_i, ev_mlstm_b_f, ev_mlstm_norm_g,
              ev_w_out, od_w_in, od_moba_qn_g, od_moba_kn_g, od_ssd_conv_w, od_ssd_conv_b,
              od_ssd_dt_bias, od_ssd_A_log, od_ssd_D, od_ssd_norm_g, od_w_out,
              moe_w_group, moe_b_group, moe_w_router, moe_b_router, moe_w_gate, moe_w_up,
              moe_w_down, ple_w_proj, ple_w_gate, ple_gate_norm_g, ple_out_norm_g):
    for i in range(DEPTH):
        h = rms_norm(x, norm1_g[i])
        if i % 2 == 0:
            j = i // 2
            mix = even_mixers(h, ev_w_in[j], ev_fox_b_f[j], ev_fox_qn_g[j], ev_fox_kn_g[j],
                              ev_mlstm_conv_w[j], ev_mlstm_conv_b[j], ev_mlstm_b_i[j],
                              ev_mlstm_b_f[j], ev_mlstm_norm_g[j])
            x = x + jnp.einsum('bse,ed->bsd', mix, ev_w_out[j])
        else:
            j = i // 2
            mix = odd_mixers(h, od_w_in[j], od_moba_qn_g[j], od_moba_kn_g[j], od_ssd_conv_w[j],
                             od_ssd_conv_b[j], od_ssd_dt_bias[j], od_ssd_A_log[j], od_ssd_D[j],
                             od_ssd_norm_g[j])
            x = x + jnp.einsum('bse,ed->bsd', mix, od_w_out[j])
        x = x + hier_moe(rms_norm(x, norm2_g[i]), moe_w_group[i], moe_b_group[i],
                         moe_w_router[i], moe_b_router[i], moe_w_gate[i], moe_w_up[i],
                         moe_w_down[i])
        x = x + per_layer_embedding(x, p[i], ple_w_proj[i], ple_w_gate[i],
                                    ple_gate_norm_g[i], ple_out_norm_g[i])
    return x
```

```python
import math
import numpy as np
from contextlib import ExitStack
import concourse.bass as bass
import concourse.mybir as mybir
from concourse.bass_utils import run_bass_kernel_spmd

dt = mybir.dt
F32 = dt.float32
BF16 = dt.bfloat16
AF = mybir.ActivationFunctionType
ALU = mybir.AluOpType
AX = mybir.AxisListType

NCORES = 8
SAME_ENGINE_SYNC = True
DBG = {"A", "B", "C", "D", "D1", "D2", "E", "F"}
TOK = 4096
SEQ = 2048
NTS = 16
EPS = 1e-6
EVC = 4112
ODC = 3080


class Unit:
    __slots__ = ("w", "r")

    def __init__(self):
        self.w = None
        self.r = {}


class V:
    __slots__ = ("ap", "units")

    def __init__(self, ap, units):
        self.ap = ap
        self.units = units

    def __getitem__(self, idx):
        return V(self.ap[idx], self.units)

    def rr(self, s, **kw):
        return V(self.ap.rearrange(s, **kw), self.units)

    def bc(self, shape):
        return V(self.ap.to_broadcast(list(shape)), self.units)

    def ub(self, axis, n):
        a = self.ap.unsqueeze(axis)
        shp = list(a.shape)
        shp[axis] = n
        return V(a.to_broadcast(shp), self.units)

    def bitcast(self, d):
        return V(self.ap.bitcast(d), self.units)

    @property
    def shape(self):
        return self.ap.shape


class T:
    def __init__(self, ap, nparts=1):
        self.ap = ap
        self.us = [Unit() for _ in range(nparts)]

    def __getitem__(self, idx):
        return V(self.ap[idx], self.us)

    def u(self, i):
        if isinstance(i, int):
            return V(self.ap, [self.us[i]])
        return V(self.ap, [self.us[j] for j in i])

    def all(self):
        return V(self.ap, self.us)


class K:
    def __init__(self, dma_ring=8):
        self.nc = bass.Bass("TRN2", target_bir_lowering=False)
        self.es = ExitStack()
        nc = self.nc
        self.eng = {"pe": nc.tensor, "dve": nc.vector, "act": nc.scalar,
                    "pool": nc.gpsimd, "sp": nc.sync}
        self.sem = {}
        self.cnt = {}
        self.cur = {}
        self.epoch = {}
        for e in ("pe", "dve", "act", "pool"):
            self.epoch[e] = 0
            self._new_epoch(e)
        self.ring = {}
        self.ringpos = {}
        for q in ("sp", "pool"):
            self.ring[q] = []
            self.ringpos[q] = 0
            for i in range(dma_ring):
                sm = self.es.enter_context(nc.semaphore(f"d_{q}{i}"))
                self.ring[q].append(sm)
                self.sem[("d", q, i)] = sm
                self.cnt[("d", q, i)] = 0
        self.seen = {e: {} for e in ("pe", "dve", "act", "pool", "sp")}
        self.ninst = 0
        self.nwait = 0
        self.scopes = []
        self.uid = 0

    SEM_LIMIT = 30000

    def _new_epoch(self, e):
        key = ("c", e, self.epoch[e])
        self.epoch[e] += 1
        self.sem[key] = self.es.enter_context(self.nc.semaphore(f"s_{e}{key[2]}"))
        self.cnt[key] = 0
        self.cur[e] = key

    def dram(self, name, shape, dtype, kind="Internal", nparts=1):
        t = self.nc.dram_tensor(name, list(shape), dtype, kind=kind)
        return T(t.ap(), nparts)

    def _alloc(self, fn, name, shape, dtype, nparts):
        st = self.scopes[-1] if self.scopes else self.es
        self.uid += 1
        t = st.enter_context(fn(f"{name}_{self.uid}", list(shape), dtype))
        return T(t.ap(), nparts)

    def sb(self, name, shape, dtype=F32, nparts=1):
        return self._alloc(self.nc.sbuf_tensor, name, shape, dtype, nparts)

    def ps(self, name, shape, dtype=F32, nparts=1):
        return self._alloc(self.nc.psum_tensor, name, shape, dtype, nparts)

    def _need(self, e, ev):
        if ev is None:
            return
        key, val = ev
        if key[0] == "c" and key[1] == e and (e == "pe" or not SAME_ENGINE_SYNC):
            return
        if self.seen[e].get(key, 0) >= val:
            return
        self.eng[e].wait_ge(self.sem[key], val)
        self.nwait += 1
        self.seen[e][key] = val

    def _deps(self, e, reads, writes):
        for v in reads:
            for u in v.units:
                self._need(e, u.w)
        for v in writes:
            for u in v.units:
                self._need(e, u.w)
                for k2, val in u.r.items():
                    self._need(e, (k2, val))

    def _mark(self, ev, reads, writes):
        key, val = ev
        for v in reads:
            for u in v.units:
                if u.r.get(key, 0) < val:
                    u.r[key] = val
        for v in writes:
            for u in v.units:
                u.w = ev
                u.r = {}

    def op(self, e, fn, reads, writes, *args, **kw):
        self._deps(e, reads, writes)
        ins = fn(*args, **kw)
        key = self.cur[e]
        self.cnt[key] += 1
        ins.then_inc(self.sem[key], 1)
        self._mark((key, self.cnt[key]), reads, writes)
        self.ninst += 1
        if self.cnt[key] >= self.SEM_LIMIT:
            self._new_epoch(e)
        return ins

    def dma(self, out, in_, q="sp", **kw):
        ring = self.ring[q]
        i = self.ringpos[q] % len(ring)
        self.ringpos[q] += 1
        key = ("d", q, i)
        if self.cnt[key] > 0:
            self._need(q, (key, self.cnt[key]))
        self._deps(q, [in_], [out])
        ins = self.eng[q].dma_start(out=out.ap, in_=in_.ap, **kw)
        self.cnt[key] += 16
        ins.then_inc(self.sem[key], 16)
        self._mark((key, self.cnt[key]), [in_], [out])
        self.ninst += 1
        return ins

    def mm(self, out, lhsT, rhs, start=True, stop=True):
        return self.op("pe", self.nc.tensor.matmul, [lhsT, rhs], [out],
                       out.ap, lhsT.ap, rhs.ap, start=start, stop=stop)

    def tr(self, out, in_, ident):
        return self.op("pe", self.nc.tensor.transpose, [in_, ident], [out],
                       out.ap, in_.ap, ident.ap)

    def act(self, out, in_, func, bias=None, scale=None, accum_out=None):
        reads = [in_]
        writes = [out]
        kw = {}
        if bias is not None:
            if isinstance(bias, V):
                reads.append(bias)
                kw["bias"] = bias.ap
            else:
                kw["bias"] = bias
        if scale is not None:
            if isinstance(scale, V):
                reads.append(scale)
                kw["scale"] = scale.ap
            else:
                kw["scale"] = scale
        if accum_out is not None:
            writes.append(accum_out)
            kw["accum_out"] = accum_out.ap
        return self.op("act", self.nc.scalar.activation, reads, writes,
                       out.ap, in_.ap, func, **kw)

    def tt(self, out, in0, in1, op, e="dve"):
        return self.op(e, self.eng[e].tensor_tensor, [in0, in1], [out],
                       out.ap, in0.ap, in1.ap, op)

    def ts(self, out, in0, s1, s2, op0, op1=None, e="dve", accum_out=None):
        reads = [in0]
        writes = [out]
        a1, a2 = s1, s2
        if isinstance(s1, V):
            reads.append(s1)
            a1 = s1.ap
        if isinstance(s2, V):
            reads.append(s2)
            a2 = s2.ap
        kw = {}
        if op1 is not None:
            kw["op1"] = op1
        if accum_out is not None:
            writes.append(accum_out)
            kw["accum_out"] = accum_out.ap
        return self.op(e, self.eng[e].tensor_scalar, reads, writes,
                       out.ap, in0.ap, a1, a2, op0, **kw)

    def stt(self, out, in0, scalar, in1, op0, op1):
        reads = [in0, in1]
        a = scalar
        if isinstance(scalar, V):
            reads.append(scalar)
            a = scalar.ap
        return self.op("dve", self.nc.vector.scalar_tensor_tensor, reads, [out],
                       out.ap, in0.ap, a, in1.ap, op0, op1)

    def copy(self, out, in_, e="dve"):
        if e == "act":
            return self.op("act", self.nc.scalar.copy, [in_], [out], out.ap, in_.ap)
        return self.op(e, self.eng[e].tensor_copy, [in_], [out], out.ap, in_.ap)

    def memset(self, out, val, e="dve"):
        return self.op(e, self.eng[e].memset, [], [out], out.ap, val)

    def reduce(self, out, in_, op, axis=AX.X):
        return self.op("dve", self.nc.vector.tensor_reduce, [in_], [out],
                       out.ap, in_.ap, axis, op)

    def recip(self, out, in_):
        return self.op("dve", self.nc.vector.reciprocal, [in_], [out], out.ap, in_.ap)

    def max8(self, out, in_):
        return self.op("dve", self.nc.vector.max, [in_], [out], out.ap, in_.ap)

    def barrier(self):
        evs = [(key, c) for key, c in self.cnt.items() if c > 0]
        for e in ("pe", "dve", "act", "pool", "sp"):
            for key, val in evs:
                if self.seen[e].get(key, 0) >= val:
                    continue
                self.eng[e].wait_ge(self.sem[key], val)
                self.nwait += 1
                self.seen[e][key] = val

    def scope(self):
        k = self

        class _S:
            def __enter__(s):
                k.scopes.append(ExitStack())

            def __exit__(s, *a):
                k.barrier()
                k.scopes.pop().close()
                return False
        return _S()

    def finish(self):
        self.barrier()
        self.es.close()


_CL = [("ident", 128), ("tri", 128), ("ones", 128), ("negmask", 128)]
for _l in range(2):
    _CL += [(f"n1g{_l}", 1024), (f"n2g{_l}", 1024), (f"pgg{_l}", 1024), (f"pog{_l}", 1024), (f"rb{_l}", 20)]
_CL += [("fbf", 8), ("fgqk", 1024), ("mcw", 4096), ("mcb", 1024), ("mbi", 4), ("mbf", 4), ("mng", 512)]
_CL += [("ogqk", 1024), ("scw", 4096), ("scb", 1024), ("sdtb", 8), ("salog", 8), ("sD", 8), ("sng", 512)]
COFF = {}
_o = 0
for _n, _w in _CL:
    COFF[_n] = (_o, _w)
    _o += _w
NCST = _o


def make_cst(I):
    c = np.zeros((128, NCST), np.float32)

    def put(name, row):
        o, w = COFF[name]
        row = np.asarray(row, np.float32).reshape(-1)
        assert row.shape[0] == w, (name, row.shape, w)
        c[:, o:o + w] = row[None, :]

    o, w = COFF["ident"]
    c[:, o:o + w] = np.eye(128, dtype=np.float32)
    s = np.arange(128)[:, None]
    t = np.arange(128)[None, :]
    o, w = COFF["tri"]
    c[:, o:o + w] = (s <= t).astype(np.float32)
    o, w = COFF["ones"]
    c[:, o:o + w] = 1.0
    o, w = COFF["negmask"]
    c[:, o:o + w] = np.where(s <= t, 0.0, -1.0e4).astype(np.float32)
    for l in range(2):
        put(f"n1g{l}", I["norm1_g"][l])
        put(f"n2g{l}", I["norm2_g"][l])
        put(f"pgg{l}", I["ple_gate_norm_g"][l])
        put(f"pog{l}", I["ple_out_norm_g"][l])
        put(f"rb{l}", np.concatenate([I["moe_b_group"][l], I["moe_b_router"][l]]))
    put("fbf", I["ev_fox_b_f"][0])
    put("fgqk", np.concatenate([np.tile(I["ev_fox_qn_g"][0], 8), np.tile(I["ev_fox_kn_g"][0], 8)]))
    put("mcw", I["ev_mlstm_conv_w"][0].reshape(-1))
    put("mcb", I["ev_mlstm_conv_b"][0])
    put("mbi", I["ev_mlstm_b_i"][0])
    put("mbf", I["ev_mlstm_b_f"][0])
    put("mng", I["ev_mlstm_norm_g"][0])
    put("ogqk", np.concatenate([np.tile(I["od_moba_qn_g"][0], 8), np.tile(I["od_moba_kn_g"][0], 8)]))
    put("scw", I["od_ssd_conv_w"][0].reshape(-1))
    put("scb", I["od_ssd_conv_b"][0])
    put("sdtb", I["od_ssd_dt_bias"][0])
    put("salog", I["od_ssd_A_log"][0])
    put("sD", I["od_ssd_D"][0])
    put("sng", I["od_ssd_norm_g"][0])
    return c


def build(stage="full"):
    k = K()
    nc = k.nc
    EI = "ExternalInput"
    x_in = k.dram("x", [TOK, 1024], F32, EI, nparts=32)
    p_in = k.dram("p", [2, TOK, 256], F32, EI)
    ev_w_in = k.dram("ev_w_in", [1024, EVC], F32, EI)
    ev_w_out = k.dram("ev_w_out", [1024, 1024], F32, EI)
    od_w_in = k.dram("od_w_in", [1024, ODC], F32, EI)
    od_w_out = k.dram("od_w_out", [1024, 1024], F32, EI)
    moe_w_rt = k.dram("moe_w_rt", [2, 1024, 20], F32, EI)
    moe_w_gate = k.dram("moe_w_gate", [2, 16, 1024, 256], F32, EI)
    moe_w_up = k.dram("moe_w_up", [2, 16, 1024, 256], F32, EI)
    moe_w_down = k.dram("moe_w_down", [2, 16, 256, 1024], F32, EI)
    ple_w_proj = k.dram("ple_w_proj", [2, 256, 1024], F32, EI)
    ple_w_gate = k.dram("ple_w_gate", [2, 1024, 1024], F32, EI)
    cst = k.dram("cst", [128, NCST], F32, EI)
    blkind = k.dram("blkind", [8, SEQ], F32, EI)

    EO = "ExternalOutput"
    MIX = k.dram("mix", [TOK, 1024], F32, EO if stage in ("L0mix", "L1mix") else "Internal", nparts=32)
    XB = k.dram("xb", [TOK, 1024], F32, EO if stage in ("L0", "POST") else "Internal", nparts=32)
    OUT = k.dram("out", [TOK, 1024], F32, EO, nparts=32)
    GF = k.dram("gf", [TOK, 512], F32, nparts=32)
    UM = k.dram("um", [2, 3 + SEQ, 2056], F32, nparts=34)

    def crow(name, c0=0, c1=None, dtype=F32):
        o, w = COFF[name]
        if c1 is None:
            c1 = w
        t = k.sb("c_" + name, [128, c1 - c0], F32)
        k.dma(t[:], cst[:, o + c0:o + c1])
        return t

    ident_f = crow("ident")
    tri_f = crow("tri")
    ones_f = crow("ones")
    ident_b = k.sb("ident_b", [128, 128], BF16)
    tri_b = k.sb("tri_b", [128, 128], BF16)
    ones_b = k.sb("ones_b", [128, 128], BF16)
    k.copy(ident_b[:], ident_f[:])
    k.copy(tri_b[:], tri_f[:])
    k.copy(ones_b[:], ones_f[:])
    P = [k.ps(f"P{i}", [128, 512], F32, nparts=1) for i in range(8)]

    def pq(b, q):
        return P[b].all()[:, q * 128:(q + 1) * 128]

    def pbf(b):
        return P[b].all().bitcast(BF16)

    ctr = {"cast": 0}

    def cast_rr(out, in_):
        e = ("pool", "dve", "act")[ctr["cast"] % 3]
        ctr["cast"] += 1
        k.copy(out, in_, e=e)

    def load_w_bf16(dst, src_ap_fn, nkc, ncols, stage_tiles):
        for kc in range(nkc):
            st = stage_tiles[kc % len(stage_tiles)]
            k.dma(st[:, 0:ncols], src_ap_fn(kc))
            cast_rr(dst[:, kc, :], st[:, 0:ncols])

    def act_rstd(v, n):
        k.act(v, v, AF.Ln, bias=EPS, scale=1.0 / n)
        k.act(v, v, AF.Exp, scale=-0.5)

    def act_sigmoid(out, in_):
        k.act(out, in_, AF.Exp, scale=-1.0)
        k.act(out, out, AF.Ln, bias=1.0)
        k.act(out, out, AF.Exp, scale=-1.0)

    def rms_rstd(out_rstd, src, n, junk):
        k.act(junk, src, AF.Square, accum_out=out_rstd)
        act_rstd(out_rstd, n)

    def transpose_to(dstT, src_b, nblk, banks, ident, bf=True):
        for g in range((nblk + 3) // 4):
            nb = min(4, nblk - g * 4)
            bank = banks[g % len(banks)]
            pv = pbf(bank) if bf else P[bank].all()
            for q in range(nb):
                b = g * 4 + q
                k.tr(pv[:, q * 128:(q + 1) * 128], src_b[:, b * 128:(b + 1) * 128], ident[:])
            k.copy(dstT[:, g * 4:g * 4 + nb, :],
                   pv[:, 0:nb * 128].rr("p (a b) -> p a b", a=nb),
                   e=("act" if g % 2 else "dve"))

    def phaseA_pipeline(front, proj_units, post_units):
        front(0)
        for u_ in proj_units(0):
            u_()
        for i in range(NTS):
            if i + 1 < NTS:
                front(i + 1)
                pu = proj_units(i + 1)
            else:
                pu = []
            po = post_units(i)
            n_po, n_pu = len(po), len(pu)
            done_pu = 0
            for a, p_ in enumerate(po):
                p_()
                want = ((a + 1) * n_pu) // n_po
                while done_pu < want:
                    pu[done_pu]()
                    done_pu += 1
            while done_pu < n_pu:
                pu[done_pu]()
                done_pu += 1

    def run_interleaved(ga, gb, ratio):
        a_done = b_done = False
        while not (a_done and b_done):
            for _ in range(ratio):
                if a_done:
                    break
                try:
                    next(ga)
                except StopIteration:
                    a_done = True
            if not b_done:
                try:
                    next(gb)
                except StopIteration:
                    b_done = True

    def attention(qT_of, kT_of, Vaug, bias_of, emit_out, banksS, banksO):
        NG = len(banksS)
        PTg = [k.sb(f"PT{g}", [128, 512], BF16) for g in range(NG)]
        PT = [[PTg[g][:, q * 128:(q + 1) * 128] for q in range(4)] for g in range(NG)]
        rden = k.sb("rden", [128, 8, 1])
        oas = [k.sb(f"oa{n}", [128, 512]) for n in range(2)]
        for i in range(NTS):
            oa = oas[i % 2]
            items = [(h, j) for h in range(8) for j in range(i + 1)]
            groups = [items[a:a + 4] for a in range(0, len(items), 4)]
            pob = banksO[(i % 2) * 2:(i % 2) * 2 + 2]
            biasfn = bias_of(i)

            def po(h):
                return P[pob[h // 4]].all()[:, (h % 4) * 65:(h % 4) * 65 + 65]

            def stage1(g):
                bank = banksS[g % NG]
                for q, (h, j) in enumerate(groups[g]):
                    k.mm(pq(bank, q), kT_of(h, j), qT_of(h, i))
                if biasfn is None:
                    nq = len(groups[g])
                    k.act(PTg[g % NG][:, 0:nq * 128], P[bank].all()[:, 0:nq * 128], AF.Exp, scale=0.125)
                for q, (h, j) in enumerate(groups[g]):
                    pt = PT[g % NG][q]
                    if biasfn is not None:
                        k.act(pt, pq(bank, q), AF.Exp, bias=biasfn(h, j), scale=0.125)
                    if j == i:
                        k.tt(pt, pt, tri_b[:], ALU.mult)

            def stage2(g):
                for q, (h, j) in enumerate(groups[g]):
                    k.mm(po(h), PT[g % NG][q], Vaug.u(j)[:, j, h * 65:(h + 1) * 65],
                         start=(j == 0), stop=(j == i))
                    if j == i and h % 4 == 3:
                        hb = h // 4
                        pb = P[pob[hb]].all()[:, 0:260].rr("p (h e) -> p h e", e=65)
                        k.recip(rden[:, hb * 4:(hb + 1) * 4, :], pb[:, :, 64:65])
                        k.tt(oa[:, hb * 256:(hb + 1) * 256].rr("p (h d) -> p h d", d=64), pb[:, :, 0:64],
                             rden[:, hb * 4:(hb + 1) * 4, :].bc([128, 4, 64]), ALU.mult)

            LOOK = NG - 1
            N = len(groups)
            for g in range(min(LOOK, N)):
                stage1(g)
            for g in range(N):
                if g + LOOK < N:
                    stage1(g + LOOK)
                stage2(g)
                yield
            emit_out(i, oa)
            yield

    def qk_headnorm(u, sq, ssqk, qkb, gqk):
        k.act(sq[:], u[:, 0:1024], AF.Square)
        k.reduce(ssqk[:], sq[:].rr("p (h d) -> p h d", d=64), ALU.add)
        act_rstd(ssqk[:], 64)
        k.tt(sq[:].rr("p (h d) -> p h d", d=64), u[:, 0:1024].rr("p (h d) -> p h d", d=64),
             ssqk[:].ub(2, 64), ALU.mult)
        k.tt(qkb[:], sq[:], gqk[:], ALU.mult)

    def logsig(out, in_, tmp):
        k.act(tmp, in_, AF.Exp, scale=-1.0)
        k.act(tmp, tmp, AF.Ln, bias=1.0)
        k.ts(out, tmp, -1.0, None, ALU.mult)

    def conv_part1(acc, ws, w3v, cw, cb, t1s):
        srcs = [ws[0][:], ws[1][:], ws[2][:], w3v]
        engs = ["pool", "dve", "pool", "dve"]
        for j in range(4):
            k.tt(t1s[j][:], srcs[j], cw[:, j * 1024:(j + 1) * 1024], ALU.mult, e=engs[j])
        k.tt(acc[:], t1s[1][:], cb[:], ALU.add)
        k.tt(acc[:], acc[:], t1s[3][:], ALU.add)
        k.tt(t1s[0][:], t1s[0][:], t1s[2][:], ALU.add, e="pool")
        k.tt(acc[:], acc[:], t1s[0][:], ALU.add)

    def conv_part2(dst, acc, sig):
        act_sigmoid(sig[:], acc[:])
        k.tt(dst, acc[:], sig[:], ALU.mult)

    def zero_um_pad():
        z = k.sb("zpad", [128, 2056])
        k.memset(z[:], 0.0)
        for s in range(2):
            k.dma(UM.u(s * 17)[s, 0:3, :], z[0:3, :], q="pool")

    def layer0_mixers(X):
        with k.scope():
            zero_um_pad()
        for s in range(2):
            with k.scope():
                qT = k.sb("qT", [64, 8, SEQ], BF16, nparts=NTS)
                kT = k.sb("kT", [64, 8, SEQ], BF16, nparts=NTS)
                Vaug = k.sb("Vaug", [128, NTS, 520], BF16, nparts=NTS)
                CF = k.sb("CF", [128, NTS, 8])
                CFE = k.sb("CFE", [128, NTS, 8])
                k.memset(Vaug.all()[:, :, :], 1.0)
                with k.scope():
                    wb = k.sb("wb_in", [128, 8, EVC], BF16)
                    with k.scope():
                        stg = [k.sb(f"stg{n}", [128, EVC]) for n in range(2)]
                        load_w_bf16(wb, lambda kc: ev_w_in[kc * 128:(kc + 1) * 128, :], 8, EVC, stg)
                    n1g = crow("n1g0")
                    gqk = crow("fgqk")
                    fbf = crow("fbf")
                    xts = [k.sb(f"xt{n}", [128, 1024]) for n in range(1)]
                    junk = k.sb("junk", [128, 1024])
                    hbs = [k.sb(f"hb{n}", [128, 1024], BF16) for n in range(2)]
                    hTs = [k.sb(f"hT{n}", [128, 8, 128], BF16) for n in range(2)]
                    UA = 2560
                    uas = [k.sb(f"ua{n}", [128, UA]) for n in range(2)]
                    ub_ = k.sb("ub", [128, EVC - UA])
                    rss = [k.sb(f"rs{n}", [128, 1]) for n in range(2)]
                    ssqk = k.sb("ssqk", [128, 16])
                    qkb = k.sb("qkb", [128, 1024], BF16)
                    t8 = k.sb("t8", [128, 8])
                    lf = k.sb("lf", [128, 8])
                    LFacc = k.sb("LFacc", [128, 8])
                    gt = k.sb("gt", [128, 512])

                    def U(i, c0, c1):
                        if c1 <= UA:
                            return uas[i % 2][:, c0:c1]
                        assert c0 >= UA
                        return ub_[:, c0 - UA:c1 - UA]

                    def front(i):
                        ti = s * NTS + i
                        r0 = ti * 128
                        xt, hb, hT, rs = xts[0], hbs[i % 2], hTs[i % 2], rss[i % 2]
                        k.dma(xt[:], X.u(ti)[r0:r0 + 128, :])
                        rms_rstd(rs[:], xt[:], 1024, junk[:])
                        k.stt(hb[:], xt[:], rs[:], n1g[:], ALU.mult, ALU.mult)
                        transpose_to(hT, hb, 8, [0, 1], ident_b)

                    def proj_units(i):
                        hT = hTs[i % 2]

                        def chunk(c):
                            c0 = c * 512
                            cw_ = min(512, EVC - c0)
                            pb = P[2 + c % 4].all()
                            for kc in range(8):
                                k.mm(pb[:, 0:cw_], hT[:, kc, :], wb[:, kc, c0:c0 + cw_],
                                     start=(kc == 0), stop=(kc == 7))
                            k.copy(U(i, c0, c0 + cw_), pb[:, 0:cw_], e=("act" if c % 2 else "dve"))
                        return [(lambda c=c: chunk(c)) for c in range(9)]

                    def post_units(i):
                        ti = s * NTS + i
                        r0 = ti * 128
                        uq = U(i, 0, 1024)

                        def p_um():
                            k.dma(UM.u(s * 17 + 1 + i)[s, 3 + i * 128:3 + (i + 1) * 128, UA - 2056:2056],
                                  U(i, UA, EVC), q="pool")
                            k.dma(UM.u(s * 17 + 1 + i)[s, 3 + i * 128:3 + (i + 1) * 128, 0:UA - 2056],
                                  U(i, 2056, UA), q="pool")

                        def p_norm1():
                            k.act(junk[:], uq, AF.Square)
                            k.reduce(ssqk[:], junk[:].rr("p (h d) -> p h d", d=64), ALU.add)
                            act_rstd(ssqk[:], 64)

                        def p_norm2():
                            k.tt(junk[:].rr("p (h d) -> p h d", d=64), uq.rr("p (h d) -> p h d", d=64),
                                 ssqk[:].ub(2, 64), ALU.mult)
                            k.tt(qkb[:], junk[:], gqk[:], ALU.mult)

                        def p_tr(half):
                            dst = (qT, kT)[half]
                            pv = pbf(half)
                            for h in range(8):
                                c0 = half * 512 + h * 64
                                k.tr(pv[0:64, h * 128:(h + 1) * 128], qkb[:, c0:c0 + 64], ident_b[:])
                            k.copy(dst.u(i)[0:64, :, i * 128:(i + 1) * 128],
                                   pv[0:64, 0:1024].rr("p (a b) -> p a b", a=8),
                                   e=("act" if half else "dve"))

                        def p_v():
                            k.copy(Vaug.u(i)[:, i, :].rr("p (h e) -> p h e", e=65)[:, :, 0:64],
                                   U(i, 1024, 1536).rr("p (h d) -> p h d", d=64), e="pool")

                        def p_lf():
                            k.tt(t8[:], U(i, 1536, 1544), fbf[:], ALU.add)
                            logsig(lf[:], t8[:], t8[:])

                        def p_cf():
                            pc = P[6].all()
                            k.mm(pc[:, 0:8], tri_f[:], lf[:], start=True, stop=(i == 0))
                            if i > 0:
                                k.mm(pc[:, 0:8], ones_f[:], LFacc[:], start=False, stop=True)
                            k.copy(CF[:, i, :], pc[:, 0:8])
                            if i == 0:
                                k.copy(LFacc[:], lf[:])
                            else:
                                k.tt(LFacc[:], LFacc[:], lf[:], ALU.add)
                            k.mm(pc[:, 8:16], ones_f[:], LFacc[:], start=True, stop=True)
                            k.copy(CFE[:, i, :], pc[:, 8:16])

                        def p_gate():
                            act_sigmoid(gt[:], U(i, 1544, 2056))
                            k.dma(GF.u(ti)[r0:r0 + 128, :], gt[:], q="pool")

                        return [p_um, p_norm1, p_norm2, lambda: p_tr(0), lambda: p_tr(1), p_v, p_lf, p_cf, p_gate]

                    phaseA_pipeline(front, proj_units, post_units)
                with k.scope():
                    NBs = [k.sb(f"NB{n}", [128, NTS, 8]) for n in range(2)]
                    gts = [k.sb(f"gts{n}", [128, 512]) for n in range(2)]

                    def bias_of(i):
                        NB = NBs[i % 2]
                        k.tt(NB[:, 0:i + 1, :], CFE[:, i:i + 1, :].bc([128, i + 1, 8]),
                             CF[:, 0:i + 1, :], ALU.subtract)
                        return lambda h, j: NB[:, j, h:h + 1]

                    def emit(i, oa):
                        ti = s * NTS + i
                        r0 = ti * 128
                        g = gts[i % 2]
                        k.dma(g[:], GF.u(ti)[r0:r0 + 128, :])
                        k.tt(oa[:], oa[:], g[:], ALU.mult)
                        k.dma(MIX.u(ti)[r0:r0 + 128, 0:512], oa[:], q="pool")

                    genB = attention(lambda h, i: qT.u(i)[0:64, h, i * 128:(i + 1) * 128],
                                     lambda h, j: kT.u(j)[0:64, h, j * 128:(j + 1) * 128],
                                     Vaug, bias_of, emit, [0, 1], [2, 3, 2, 3])
                    genC = mlstm_phase(s)
                    run_interleaved(genB, genC, 2)

    def mlstm_phase(s):
        cw = crow("mcw")
        cb = crow("mcb")
        mbi = crow("mbi")
        mbf = crow("mbf")
        mng = crow("mng")
        w3s = [k.sb(f"w3{n}", [128, 2056]) for n in range(2)]
        wss = [[k.sb(f"ws{n}_{j}", [128, 1024]) for j in range(3)] for n in range(2)]
        accs = [k.sb(f"cacc{n}", [128, 1024]) for n in range(2)]
        t1s = [k.sb(f"ct1_{n}", [128, 1024]) for n in range(4)]
        csig = k.sb("csig", [128, 1024])
        qk = k.sb("mqk", [128, 1024])
        g8 = k.sb("g8", [128, 4])
        lfm = k.sb("lfm", [128, 4])
        ic = k.sb("ic", [128, 4])
        qs = k.sb("qs", [128, 4])
        ks = k.sb("ks", [128, 4])
        ebl = k.sb("ebl", [128, 4])
        qsb = k.sb("qsb", [128, 4, 128], BF16)
        ksb = k.sb("ksb", [128, 4, 128], BF16)
        qTt = k.sb("qTt", [128, 4, 128], BF16)
        kTt = k.sb("kTt", [128, 4, 128], BF16)
        va = k.sb("va", [128, 4, 129], BF16)
        wTs = [k.sb(f"wT{n}", [128, 128], BF16) for n in range(2)]
        Uf = k.sb("Uf", [128, 4, 129], nparts=4)
        Ub = k.sb("Ub", [128, 4, 129], BF16, nparts=4)
        den = k.sb("mden", [128, 4])
        hc = k.sb("hc", [128, 4, 128])
        sqh = k.sb("sqh", [128, 512])
        ssh = k.sb("ssh", [128, 4])
        sg = k.sb("msg", [128, 512])
        k.memset(Uf.all()[:, :, :], 0.0)
        k.memset(Ub.all()[:, :, :], 0.0)
        k.memset(va[:, :, 128:129], 1.0)

        def front(i):
            r = 3 + i * 128
            uu = s * 17 + 1 + i
            k.dma(w3s[i % 2][:], UM.u(uu)[s, r:r + 128, :])
            for j in range(3):
                k.dma(wss[i % 2][j][:], UM.u([uu - 1, uu])[s, r - 3 + j:r - 3 + j + 128, 0:1024])
            conv_part1(accs[i % 2], wss[i % 2], w3s[i % 2][:, 0:1024], cw, cb, t1s)

        front(0)
        for i in range(NTS):
            ti = s * NTS + i
            r0 = ti * 128
            w3 = w3s[i % 2]
            if i + 1 < NTS:
                front(i + 1)
            yield
            conv_part2(qk[:], accs[i % 2], csig)
            yield
            k.tt(g8[:], w3[:, 1540:1544], mbf[:], ALU.add)
            logsig(lfm[:], g8[:], g8[:])
            k.tt(ic[:], w3[:, 1536:1540], mbi[:], ALU.add)
            pc = P[7].all()
            k.mm(pc[:, 0:4], tri_f[:], lfm[:])
            k.mm(pc[:, 4:8], ones_f[:], lfm[:])
            k.act(qs[:], pc[:, 0:4], AF.Exp)
            k.tt(g8[:], ic[:], pc[:, 0:4], ALU.subtract)
            k.act(ks[:], g8[:], AF.Exp, bias=-0.5 * math.log(128.0))
            k.act(ebl[:], pc[:, 4:8], AF.Exp)
            yield
            k.tt(qsb[:], qk[:, 0:512].rr("p (h d) -> p h d", d=128), qs[:].ub(2, 128), ALU.mult)
            k.tt(ksb[:], qk[:, 512:1024].rr("p (h d) -> p h d", d=128), ks[:].ub(2, 128), ALU.mult)
            transpose_to(qTt, qsb[:].rr("p h d -> p (h d)"), 4, [4], ident_b)
            transpose_to(kTt, ksb[:].rr("p h d -> p (h d)"), 4, [4], ident_b)
            k.copy(va[:, :, 0:128], w3[:, 1024:1536].rr("p (h d) -> p h d", d=128), e="pool")
            yield
            for h in range(4):
                pss = pq(5, 0)
                k.mm(pss, kTt[:, h, :], qTt[:, h, :])
                wT = wTs[h % 2]
                k.tt(wT[:], pss, tri_f[:], ALU.mult)
                pn = P[6].all()[:, 0:129]
                k.mm(pn, wT[:], va[:, h, :], start=True, stop=False)
                k.mm(pn, qTt[:, h, :], Ub.u(h)[:, h, :], start=False, stop=True)
                pu = P[7].all()[:, 0:129]
                k.mm(pu, ksb[:, h, :], va[:, h, :])
                k.tt(Uf.u(h)[:, h, :], Uf.u(h)[:, h, :], pu, ALU.add)
                k.ts(Uf.u(h)[:, h, :], Uf.u(h)[:, h, :], ebl[:, h:h + 1], None, ALU.mult)
                k.copy(Ub.u(h)[:, h, :], Uf.u(h)[:, h, :], e="pool")
                k.act(den[:, h:h + 1], pn[:, 128:129], AF.Abs)
                k.ts(den[:, h:h + 1], den[:, h:h + 1], 1.0, None, ALU.max)
                k.recip(den[:, h:h + 1], den[:, h:h + 1])
                k.ts(hc[:, h, :], pn[:, 0:128], den[:, h:h + 1], None, ALU.mult)
                yield
            hcf = hc[:].rr("p h d -> p (h d)")
            k.act(sqh[:], hcf, AF.Square)
            k.reduce(ssh[:], sqh[:].rr("p (h d) -> p h d", d=128), ALU.add)
            act_rstd(ssh[:], 128)
            k.tt(hc[:], hc[:], ssh[:].ub(2, 128), ALU.mult)
            k.tt(hcf, hcf, mng[:], ALU.mult)
            act_sigmoid(sg[:], w3[:, 1544:2056])
            k.tt(sqh[:], hcf, sg[:], ALU.mult)
            k.dma(MIX.u(ti)[r0:r0 + 128, 512:1024], sqh[:], q="pool")
            yield

    def layer1_mixers(X):
        with k.scope():
            zero_um_pad()
        for s in range(2):
            with k.scope():
                qTa = k.sb("qTa", [72, 8, SEQ], BF16, nparts=NTS)
                kTa = k.sb("kTa", [72, 8, SEQ], BF16, nparts=NTS)
                Vaug = k.sb("Vaug1", [128, NTS, 520], BF16, nparts=NTS)
                k.memset(Vaug.all()[:, :, :], 1.0)
                with k.scope():
                    wb = k.sb("wb_in1", [128, 8, ODC], BF16)
                    with k.scope():
                        stg = [k.sb(f"stg1_{n}", [128, ODC]) for n in range(2)]
                        load_w_bf16(wb, lambda kc: od_w_in[kc * 128:(kc + 1) * 128, :], 8, ODC, stg)
                    with k.scope():
                        bis = k.sb("bis", [72, SEQ])
                        k.dma(bis[64:72, :], blkind[:, :])
                        for h in range(8):
                            k.copy(kTa.all()[64:72, h, :], bis[64:72, :], e=("act" if h % 2 else "dve"))
                    n1g = crow("n1g1")
                    gqk = crow("ogqk")
                    xts = [k.sb(f"xt{n}", [128, 1024]) for n in range(2)]
                    junk = k.sb("junk", [128, 1024])
                    hbs = [k.sb(f"hb{n}", [128, 1024], BF16) for n in range(2)]
                    hTs = [k.sb(f"hT{n}", [128, 8, 128], BF16) for n in range(2)]
                    u = k.sb("u", [128, ODC])
                    rs = k.sb("rs", [128, 1])
                    ssqk = k.sb("ssqk", [128, 16])
                    qkb = k.sb("qkb", [128, 1024], BF16)
                    KS = k.sb("KS", [64, 8, NTS])
                    kmf = k.sb("kmf", [64, 8, 8])
                    kmT = k.sb("kmT", [64, 8, 8], BF16)
                    G = k.sb("G", [128, 8, 8])
                    m8 = k.sb("m8", [128, 8, 8])
                    MB = k.sb("MB", [128, 8, 8])
                    MBb = k.sb("MBb", [128, 64], BF16)
                    MBT = k.sb("MBT", [8, 8, 128], BF16)
                    k.memset(kmT[:], 0.0)
                    for i in range(NTS):
                        ti = s * NTS + i
                        r0 = ti * 128
                        own = i // 2
                        xt, hb, hT = xts[i % 2], hbs[i % 2], hTs[i % 2]
                        k.dma(xt[:], X.u(ti)[r0:r0 + 128, :])
                        rms_rstd(rs[:], xt[:], 1024, junk[:])
                        k.stt(hb[:], xt[:], rs[:], n1g[:], ALU.mult, ALU.mult)
                        transpose_to(hT, hb, 8, [0, 1], ident_b)
                        for c in range(7):
                            c0 = c * 512
                            cw_ = min(512, ODC - c0)
                            pb = P[2 + c % 4].all()
                            for kc in range(8):
                                k.mm(pb[:, 0:cw_], hT[:, kc, :], wb[:, kc, c0:c0 + cw_],
                                     start=(kc == 0), stop=(kc == 7))
                            k.copy(u[:, c0:c0 + cw_], pb[:, 0:cw_], e=("act" if c % 2 else "dve"))
                        qk_headnorm(u, junk, ssqk, qkb, gqk)
                        for half, dst in ((0, qTa), (1, kTa)):
                            pv = pbf(half)
                            for h in range(8):
                                c0 = half * 512 + h * 64
                                k.tr(pv[0:64, h * 128:(h + 1) * 128], qkb[:, c0:c0 + 64], ident_b[:])
                            k.copy(dst.u(i)[0:64, :, i * 128:(i + 1) * 128],
                                   pv[0:64, 0:1024].rr("p (a b) -> p a b", a=8),
                                   e=("act" if half else "dve"))
                        k.copy(Vaug.u(i)[:, i, :].rr("p (h e) -> p h e", e=65)[:, :, 0:64],
                               u[:, 1024:1536].rr("p (h d) -> p h d", d=64), e="pool")
                        pk = P[6].all()
                        for h in range(8):
                            k.mm(pk[0:64, h:h + 1], qkb[:, 512 + h * 64:512 + (h + 1) * 64], ones_b[:, 0:1])
                        k.copy(KS[:, :, i], pk[0:64, 0:8])
                        if own >= 1:
                            pg = P[7].all()
                            for h in range(8):
                                k.mm(pg[:, h * 8:(h + 1) * 8], qTa.u(i)[0:64, h, i * 128:(i + 1) * 128], kmT[:, h, :])
                            k.memset(G[:], -1.0e30)
                            k.copy(G[:, :, 0:own], pg[:, 0:64].rr("p (h n) -> p h n", n=8)[:, :, 0:own])
                            for h in range(8):
                                k.max8(m8[:, h, :], G[:, h, :])
                            k.tt(MB[:], G[:], m8[:, :, 2:3].bc([128, 8, 8]), ALU.is_lt)
                            k.ts(MB[:], MB[:], -4096.0, None, ALU.mult)
                            if own < 8:
                                k.memset(MB[:, :, own:8], 0.0)
                            k.copy(MBb[:], MB[:].rr("p h n -> p (h n)"))
                        else:
                            k.memset(MBb[:], 0.0)
                        pm = pbf(7)
                        for h in range(8):
                            k.tr(pm[0:8, h * 128:(h + 1) * 128], MBb[:, h * 8:(h + 1) * 8], ident_b[:])
                        k.copy(MBT[:], pm[0:8, 0:1024].rr("p (a b) -> p a b", a=8))
                        k.dma(qTa.u(i)[64:72, :, i * 128:(i + 1) * 128], MBT[:], q="pool")
                        if i % 2 == 1:
                            k.tt(kmf[:, :, own], KS[:, :, i - 1], KS[:, :, i], ALU.add)
                            k.ts(kmT[:, :, own], kmf[:, :, own], 1.0 / 256, None, ALU.mult)
                        k.dma(UM.u(s * 17 + 1 + i)[s, 3 + i * 128:3 + (i + 1) * 128, 0:1544],
                              u[:, 1536:ODC], q="pool")
                with k.scope():
                    def emit(i, oa):
                        ti = s * NTS + i
                        r0 = ti * 128
                        k.dma(MIX.u(ti)[r0:r0 + 128, 0:512], oa[:], q="pool")

                    genB = attention(lambda h, i: qTa.u(i)[0:72, h, i * 128:(i + 1) * 128],
                                     lambda h, j: kTa.u(j)[0:72, h, j * 128:(j + 1) * 128],
                                     Vaug, lambda i: None, emit, [0, 1], [2, 3, 2, 3])
                    genC = ssd_phase(s)
                    run_interleaved(genB, genC, 2)

    def ssd_phase(s):
        cw = crow("scw")
        cb = crow("scb")
        dtb = crow("sdtb")
        alog = crow("salog")
        Dsk = crow("sD")
        sng = crow("sng")
        negm = crow("negmask")
        w3s = [k.sb(f"sw3{n}", [128, 1544]) for n in range(2)]
        wss = [[k.sb(f"sws{n}_{j}", [128, 1024]) for j in range(3)] for n in range(2)]
        accs = [k.sb(f"sacc{n}", [128, 1024]) for n in range(2)]
        t1s = [k.sb(f"st1_{n}", [128, 1024]) for n in range(4)]
        csig = k.sb("ssig", [128, 1024])
        xbc = k.sb("xbc", [128, 1024])
        Aex = k.sb("Aex", [128, 8])
        dtt = k.sb("dtt", [128, 8])
        adt = k.sb("adt", [128, 8])
        adtb = k.sb("adtb", [128, 128])
        bcs = k.sb("bcs", [128, 8])
        eb = k.sb("eb", [128, 8])
        ebl = k.sb("sebl", [128, 8])
        dec = k.sb("dec", [128, 8])
        xdt = k.sb("xdt", [128, 8, 64])
        xdtb = k.sb("xdtb", [128, 8, 64], BF16)
        xdd = k.sb("xdd", [128, 8, 64], BF16)
        BCb = k.sb("BCb", [128, 512], BF16)
        BCT = k.sb("BCT", [128, 4, 128], BF16)
        GT = k.sb("GT", [128, 2, 128])
        tmpL = [k.sb(f"tmpL{n}", [128, 128]) for n in range(2)]
        LT = [k.sb(f"LT{n}", [128, 128]) for n in range(2)]
        WT = [k.sb(f"sWT{n}", [128, 128], BF16) for n in range(2)]
        Hf = k.sb("Hf", [128, 8, 64])
        Hb = k.sb("Hb", [128, 8, 64], BF16)
        y1 = k.sb("y1", [128, 512])
        y2 = k.sb("y2", [128, 512])
        sz = k.sb("sz", [128, 512])
        ssg = k.sb("ssg", [128, 2])
        k.memset(Hf[:], 0.0)
        k.memset(Hb[:], 0.0)
        k.act(Aex[:], alog[:], AF.Exp)

        def front(i):
            r = 3 + i * 128
            uu = s * 17 + 1 + i
            k.dma(w3s[i % 2][:], UM.u(uu)[s, r:r + 128, 0:1544])
            for j in range(3):
                k.dma(wss[i % 2][j][:], UM.u([uu - 1, uu])[s, r - 3 + j:r - 3 + j + 128, 512:1536])
            conv_part1(accs[i % 2], wss[i % 2], w3s[i % 2][:, 512:1536], cw, cb, t1s)

        front(0)
        for i in range(NTS):
            ti = s * NTS + i
            r0 = ti * 128
            w3 = w3s[i % 2]
            if i + 1 < NTS:
                front(i + 1)
            yield
            conv_part2(xbc[:], accs[i % 2], csig)
            yield
            k.tt(dtt[:], w3[:, 1536:1544], dtb[:], ALU.add)
            k.act(dtt[:], dtt[:], AF.Exp)
            k.act(dtt[:], dtt[:], AF.Ln, bias=1.0)
            k.tt(adt[:], dtt[:], Aex[:], ALU.mult)
            k.ts(adt[:], adt[:], -1.0, None, ALU.mult)
            pc = P[4].all()
            k.mm(pc[:, 0:8], tri_f[:], adt[:])
            k.mm(pc[:, 8:16], ones_f[:], adt[:])
            k.copy(bcs[:], pc[:, 0:8])
            k.act(eb[:], pc[:, 0:8], AF.Exp)
            k.act(ebl[:], pc[:, 8:16], AF.Exp)
            k.tt(dec[:], pc[:, 8:16], bcs[:], ALU.subtract)
            k.act(dec[:], dec[:], AF.Exp)
            yield
            xs3 = xbc[:, 0:512].rr("p (h d) -> p h d", d=64)
            k.tt(xdt[:], xs3, dtt[:].ub(2, 64), ALU.mult)
            k.copy(xdtb[:], xdt[:], e="pool")
            k.tt(xdd[:], xdt[:], dec[:].ub(2, 64), ALU.mult)
            k.copy(BCb[:], xbc[:, 512:1024], e="pool")
            transpose_to(BCT, BCb, 4, [4], ident_b)
            for g in range(2):
                k.mm(pq(4, g), BCT[:, g, :], BCT[:, 2 + g, :])
            k.copy(GT[:], P[4].all()[:, 0:256].rr("p (g l) -> p g l", g=2))
            yield
            pyd = P[6].all()
            pyo = P[7].all()
            for h in range(8):
                g = h // 4
                k.copy(adtb[:], adt[:, h:h + 1].bc([128, 128]), e="pool")
                pbb = pq(5, 0)
                k.mm(pbb, adtb[:], tri_f[:])
                tl = tmpL[h % 2]
                k.stt(tl[:], pbb, bcs[:, h:h + 1], negm[:], ALU.subtract, ALU.min)
                lt = LT[h % 2]
                k.act(lt[:], tl[:], AF.Exp)
                wt = WT[h % 2]
                k.tt(wt[:], GT[:, g, :], lt[:], ALU.mult)
                k.mm(pyd[:, h * 64:(h + 1) * 64], wt[:], xdtb[:, h, :])
                k.mm(pyo[:, h * 64:(h + 1) * 64], BCT[:, 2 + g, :], Hb[:, h, :])
                if h % 2 == 1:
                    yield
            k.tt(y1[:].rr("p (h d) -> p h d", d=64), pyo[:, :].rr("p (h d) -> p h d", d=64),
                 eb[:].ub(2, 64), ALU.mult)
            k.tt(y1[:], y1[:], pyd[:, :], ALU.add)
            k.tt(y2[:].rr("p (h d) -> p h d", d=64), xs3, Dsk[:].ub(2, 64), ALU.mult)
            k.tt(y1[:], y1[:], y2[:], ALU.add)
            pdh = P[7].all()
            for h in range(8):
                g = h // 4
                k.mm(pdh[:, h * 64:(h + 1) * 64], BCb[:, g * 128:(g + 1) * 128], xdd[:, h, :])
            k.tt(Hf[:], Hf[:], ebl[:].ub(2, 64), ALU.mult)
            k.tt(Hf[:].rr("p h d -> p (h d)"), Hf[:].rr("p h d -> p (h d)"), pdh[:, :], ALU.add)
            k.copy(Hb[:], Hf[:], e="pool")
            yield
            act_sigmoid(sz[:], w3[:, 0:512])
            k.tt(sz[:], sz[:], w3[:, 0:512], ALU.mult, e="pool")
            k.tt(y1[:], y1[:], sz[:], ALU.mult)
            k.act(y2[:], y1[:], AF.Square)
            k.reduce(ssg[:], y2[:].rr("p (g d) -> p g d", d=256), ALU.add)
            act_rstd(ssg[:], 256)
            k.tt(y1[:].rr("p (g d) -> p g d", d=256), y1[:].rr("p (g d) -> p g d", d=256),
                 ssg[:].ub(2, 256), ALU.mult)
            k.tt(y2[:], y1[:], sng[:], ALU.mult)
            k.dma(MIX.u(ti)[r0:r0 + 128, 512:1024], y2[:], q="pool")
            yield

    def post_block(l, Xin, w_out, Xout):
        for s in range(2):
            with k.scope():
                h2T = k.sb("h2T", [128, 8, SEQ], BF16, nparts=NTS)
                acc = k.sb("acc", [128, NTS, 1024], nparts=NTS)
                comb = k.sb("comb", [128, NTS, 16], nparts=NTS)
                with k.scope():
                    woutb = k.sb("woutb", [128, 8, 1024], BF16)
                    stg = [k.sb(f"stgo{n}", [128, 1024]) for n in range(2)]
                    load_w_bf16(woutb, lambda kc: w_out[kc * 128:(kc + 1) * 128, :], 8, 1024, stg)
                    wrt = k.sb("wrt", [128, 8, 20])
                    for kc in range(8):
                        k.dma(wrt[:, kc, :], moe_w_rt.all()[l, kc * 128:(kc + 1) * 128, :])
                    n2g = crow(f"n2g{l}")
                    rb = crow(f"rb{l}")
                    mts = [k.sb(f"mt{n}", [128, 1024]) for n in range(2)]
                    xts = [k.sb(f"xr{n}", [128, 1024]) for n in range(2)]
                    mb_r = [k.sb(f"mb{n}", [128, 1024], BF16) for n in range(2)]
                    mT_r = [k.sb(f"mT{n}", [128, 8, 128], BF16) for n in range(2)]
                    junk = k.sb("junk2", [128, 1024])
                    hf_r = [k.sb(f"hf{n}", [128, 1024]) for n in range(2)]
                    hb16_r = [k.sb(f"hb16{n}", [128, 1024], BF16) for n in range(2)]
                    lo16_r = [k.sb(f"lo16{n}", [128, 1024], BF16) for n in range(2)]
                    loT_r = [k.sb(f"loT{n}", [128, 8, 128], BF16) for n in range(2)]
                    whi = k.sb("whi", [128, 8, 20], BF16)
                    wlo = k.sb("wlo", [128, 8, 20], BF16)
                    k.copy(whi[:], wrt[:])
                    k.tt(wlo[:], wrt[:], whi[:], ALU.subtract)
                    sm_r = [dict(rs=k.sb("rs2", [128, 1]), lg=k.sb("lg", [128, 20]), gmax=k.sb("gmax", [128, 1]),
                                 goh=k.sb("goh", [128, 4]), ge=k.sb("ge", [128, 4]), gsum=k.sb("gsum", [128, 1]),
                                 gpen=k.sb("gpen", [128, 4]), elm=k.sb("elm", [128, 16]), m8=k.sb("m8r", [128, 8]),
                                 sel=k.sb("sel", [128, 16]), ex=k.sb("ex", [128, 16]), dn=k.sb("dn", [128, 1]))
                            for n in range(2)]
                    for i in range(NTS if "D" in DBG else 0):
                        ti = s * NTS + i
                        r0 = ti * 128
                        mt, xt = mts[i % 2], xts[i % 2]
                        mb, mT, hf, hb16, lo16, loT = (mb_r[i % 2], mT_r[i % 2], hf_r[i % 2], hb16_r[i % 2],
                                                       lo16_r[i % 2], loT_r[i % 2])
                        sm = sm_r[i % 2]
                        rs, lg, gmax, goh, ge, gsum = sm["rs"], sm["lg"], sm["gmax"], sm["goh"], sm["ge"], sm["gsum"]
                        gpen, elm, m8, sel, ex, dn = sm["gpen"], sm["elm"], sm["m8"], sm["sel"], sm["ex"], sm["dn"]
                        k.dma(mt[:], MIX.u(ti)[r0:r0 + 128, :])
                        k.dma(xt[:], Xin.u(ti)[r0:r0 + 128, :])
                        k.copy(mb[:], mt[:], e="pool")
                        transpose_to(mT, mb, 8, [0, 1], ident_b)
                        for n in range(2):
                            pb = P[2 + n].all()
                            for kc in range(8):
                                k.mm(pb[:, :], mT[:, kc, :], woutb[:, kc, n * 512:(n + 1) * 512],
                                     start=(kc == 0), stop=(kc == 7))
                            k.tt(acc.u(i)[:, i, n * 512:(n + 1) * 512], pb[:, :], xt[:, n * 512:(n + 1) * 512], ALU.add)
                        x1 = acc.u(i)[:, i, :]
                        if "D1" not in DBG:
                            continue
                        rms_rstd(rs[:], x1, 1024, junk[:])
                        k.stt(hf[:], x1, rs[:], n2g[:], ALU.mult, ALU.mult)
                        k.copy(hb16[:], hf[:], e="act")
                        k.tt(lo16[:], hf[:], hb16[:], ALU.subtract)
                        for g in range(2):
                            pv = pbf(4 + g)
                            for q in range(4):
                                b = g * 4 + q
                                k.tr(pv[:, q * 128:(q + 1) * 128], hb16[:, b * 128:(b + 1) * 128], ident_b[:])
                            k.copy(h2T.u(i)[:, g * 4:(g + 1) * 4, i * 128:(i + 1) * 128],
                                   pv[:, 0:512].rr("p (a b) -> p a b", a=4), e=("act" if g else "dve"))
                        transpose_to(loT, lo16, 8, [6, 7], ident_b)
                        if "D2" not in DBG:
                            continue
                        pr = P[6].all()
                        nmm = 0
                        for kc in range(8):
                            for (a_, w_) in ((h2T.u(i)[:, kc, i * 128:(i + 1) * 128], whi), (loT[:, kc, :], whi),
                                             (h2T.u(i)[:, kc, i * 128:(i + 1) * 128], wlo)):
                                k.mm(pr[:, 0:20], a_, w_[:, kc, :], start=(nmm == 0), stop=(nmm == 23))
                                nmm += 1
                        k.tt(lg[:], pr[:, 0:20], rb[:], ALU.add)
                        k.reduce(gmax[:], lg[:, 0:4], ALU.max)
                        k.ts(goh[:], lg[:, 0:4], gmax[:], None, ALU.is_equal)
                        k.ts(gmax[:], gmax[:], -1.0, None, ALU.mult)
                        k.act(ge[:], lg[:, 0:4], AF.Exp, bias=gmax[:], accum_out=gsum[:])
                        k.recip(gsum[:], gsum[:])
                        k.ts(gpen[:], goh[:], 1.0, 30000.0, ALU.subtract, ALU.mult)
                        k.tt(elm[:].rr("p (g e) -> p g e", e=4), lg[:, 4:20].rr("p (g e) -> p g e", e=4),
                             gpen[:].ub(2, 4), ALU.add)
                        k.max8(m8[:], elm[:])
                        k.ts(sel[:], elm[:], m8[:, 1:2], None, ALU.is_ge)
                        k.ts(gmax[:], m8[:, 0:1], -1.0, None, ALU.mult)
                        k.act(ex[:], elm[:], AF.Exp, bias=gmax[:])
                        k.tt(ex[:], ex[:], sel[:], ALU.mult)
                        k.reduce(dn[:], ex[:], ALU.add)
                        k.recip(dn[:], dn[:])
                        k.tt(dn[:], dn[:], gsum[:], ALU.mult)
                        k.ts(comb.u(i)[:, i, :], ex[:], dn[:], None, ALU.mult)
                with k.scope():
                    sg_ = [k.sb(f"sg{n}", [128, 8, 256]) for n in range(2)]
                    sd_ = k.sb("sd", [128, 2, 1024])
                    wgb = [k.sb(f"wgb{n}", [128, 8, 256], BF16) for n in range(3)]
                    wub = [k.sb(f"wub{n}", [128, 8, 256], BF16) for n in range(3)]
                    wdb = [k.sb(f"wdb{n}", [128, 2, 1024], BF16) for n in range(3)]
                    sa = [k.sb(f"sa{n}", [128, 2, 512], BF16) for n in range(2)]
                    hid = [k.sb(f"hid{n}", [128, 2, 512], BF16) for n in range(2)]
                    def load_expert(e):
                        pe_ = e % 3
                        for kc in range(8):
                            k.dma(sg_[0][:, kc, :], moe_w_gate.all()[l, e, kc * 128:(kc + 1) * 128, :])
                        cast_rr(wgb[pe_][:], sg_[0][:])
                        for kc in range(8):
                            k.dma(sg_[1][:, kc, :], moe_w_up.all()[l, e, kc * 128:(kc + 1) * 128, :])
                        cast_rr(wub[pe_][:], sg_[1][:])
                        for fc in range(2):
                            k.dma(sd_[:, fc, :], moe_w_down.all()[l, e, fc * 128:(fc + 1) * 128, :])
                        cast_rr(wdb[pe_][:], sd_[:])

                    def gu_parts(idx, e, c):
                        pe_ = e % 3
                        sav, hv = sa[idx % 2], hid[idx % 2]
                        rhs_of = lambda kc: h2T.u(range(c * 4, c * 4 + 4))[:, kc, c * 512:(c + 1) * 512]

                        def gate(f):
                            pa = P[f].all()
                            for kc in range(8):
                                k.mm(pa[:, :], wgb[pe_][:, kc, f * 128:(f + 1) * 128], rhs_of(kc),
                                     start=(kc == 0), stop=(kc == 7))
                            k.act(sav[:, f, :], pa[:, :], AF.Silu)

                        def up(f):
                            pb = P[2 + f].all()
                            for kc in range(8):
                                k.mm(pb[:, :], wub[pe_][:, kc, f * 128:(f + 1) * 128], rhs_of(kc),
                                     start=(kc == 0), stop=(kc == 7))
                            k.tt(hv[:, f, :], pb[:, :], sav[:, f, :], ALU.mult)

                        return [lambda: gate(0), lambda: gate(1), lambda: up(0), lambda: up(1)]

                    def down_parts(idx, e, c):
                        pe_ = e % 3
                        hv = hid[idx % 2]

                        def one(t, n):
                            i = c * 4 + t
                            pd = P[4 + (2 * t + n) % 4].all()
                            for f in range(2):
                                k.mm(pd[:, :], hv[:, f, t * 128:(t + 1) * 128],
                                     wdb[pe_][:, f, n * 512:(n + 1) * 512],
                                     start=(f == 0), stop=(f == 1))
                            av = acc.u(i)[:, i, n * 512:(n + 1) * 512]
                            k.stt(av, pd[:, :], comb.u(i)[:, i, e:e + 1], av, ALU.mult, ALU.add)

                        return [(lambda t=t, n=n: one(t, n)) for t in range(4) for n in range(2)]

                    if "E" in DBG:
                        steps = [(e, c) for e in range(16) for c in range(4)]
                        load_expert(0)
                        load_expert(1)
                        for idx, (e, c) in enumerate(steps):
                            gp = gu_parts(idx, e, c)
                            dp = down_parts(idx - 1, *steps[idx - 1]) if idx > 0 else []
                            for u_ in range(4):
                                gp[u_]()
                                for d_ in dp[2 * u_:2 * u_ + 2]:
                                    d_()
                            if c == 0 and e + 2 < 16:
                                load_expert(e + 2)
                        for d_ in down_parts(len(steps) - 1, *steps[-1]):
                            d_()
                with k.scope():
                    wpg = k.sb("wpg", [128, 8, 1024], BF16)
                    wpp = k.sb("wpp", [128, 2, 1024], BF16)
                    stg = [k.sb(f"stgp{n}", [128, 1024]) for n in range(2)]
                    load_w_bf16(wpg, lambda kc: ple_w_gate.all()[l, kc * 128:(kc + 1) * 128, :], 8, 1024, stg)
                    load_w_bf16(wpp, lambda kc: ple_w_proj.all()[l, kc * 128:(kc + 1) * 128, :], 2, 1024, stg)
                    pgg = crow(f"pgg{l}")
                    pog = crow(f"pog{l}")
                    junk = k.sb("junk3", [128, 1024])
                    rs_r = [k.sb(f"rs3{n}", [128, 1]) for n in range(2)]
                    rsb_r = [k.sb(f"rs3b{n}", [128, 1]) for n in range(2)]
                    hb_r = [k.sb(f"hb3{n}", [128, 1024], BF16) for n in range(2)]
                    hT_r = [k.sb(f"hT3{n}", [128, 8, 128], BF16) for n in range(2)]
                    sgt_r = [k.sb(f"sgt{n}", [128, 1024]) for n in range(2)]
                    pts = [k.sb(f"pt{n}", [128, 256]) for n in range(2)]
                    ptb_r = [k.sb(f"ptb{n}", [128, 256], BF16) for n in range(2)]
                    pT_r = [k.sb(f"pT{n}", [128, 2, 128], BF16) for n in range(2)]
                    eg_r = [k.sb(f"eg{n}", [128, 1024]) for n in range(2)]
                    xo = [k.sb(f"xo{n}", [128, 1024]) for n in range(2)]
                    for i in range(NTS if "F" in DBG else 0):
                        ti = s * NTS + i
                        r0 = ti * 128
                        x2 = acc.u(i)[:, i, :]
                        pt_ = pts[i % 2]
                        rs, rsb, hb, hT, sgt, ptb, pT, eg = (rs_r[i % 2], rsb_r[i % 2], hb_r[i % 2], hT_r[i % 2],
                                                            sgt_r[i % 2], ptb_r[i % 2], pT_r[i % 2], eg_r[i % 2])
                        k.dma(pt_[:], p_in.all()[l, r0:r0 + 128, :])
                        rms_rstd(rs[:], x2, 1024, junk[:])
                        k.stt(hb[:], x2, rs[:], pgg[:], ALU.mult, ALU.mult)
                        transpose_to(hT, hb, 8, [0, 1], ident_b)
                        for n in range(2):
                            pb = P[2 + n].all()
                            for kc in range(8):
                                k.mm(pb[:, :], hT[:, kc, :], wpg[:, kc, n * 512:(n + 1) * 512],
                                     start=(kc == 0), stop=(kc == 7))
                            act_sigmoid(sgt[:, n * 512:(n + 1) * 512], pb[:, :])
                        k.copy(ptb[:], pt_[:], e="pool")
                        transpose_to(pT, ptb, 2, [4], ident_b)
                        for n in range(2):
                            pb = P[5 + n].all()
                            for kc in range(2):
                                k.mm(pb[:, :], pT[:, kc, :], wpp[:, kc, n * 512:(n + 1) * 512],
                                     start=(kc == 0), stop=(kc == 1))
                            k.tt(eg[:, n * 512:(n + 1) * 512], pb[:, :], sgt[:, n * 512:(n + 1) * 512], ALU.mult)
                        rms_rstd(rsb[:], eg[:], 1024, junk[:])
                        k.stt(eg[:], eg[:], rsb[:], pog[:], ALU.mult, ALU.mult)
                        o = xo[i % 2]
                        k.tt(o[:], eg[:], x2, ALU.add)
                        k.dma(Xout.u(ti)[r0:r0 + 128, :], o[:], q="pool")

    if stage in ("L0mix", "L0", "full"):
        layer0_mixers(x_in)
    if stage in ("L0", "full"):
        post_block(0, x_in, ev_w_out, XB)
    if stage == "L1mix":
        layer1_mixers(x_in)
    if stage == "L1":
        layer1_mixers(x_in)
        post_block(1, x_in, od_w_out, OUT)
    if stage == "POST":
        for ti in range(32):
            k.dma(MIX.u(ti)[ti * 128:(ti + 1) * 128, :], x_in.u(ti)[ti * 128:(ti + 1) * 128, :])
        post_block(0, x_in, ev_w_out, XB)
    if stage == "full":
        layer1_mixers(XB)
        post_block(1, XB, od_w_out, OUT)
    k.finish()
    return k


_BUILD_CACHE = {}


def prep_inputs(I):
    f = lambda a: np.ascontiguousarray(np.asarray(a, dtype=np.float32))
    shared = {
        "ev_w_in": f(I["ev_w_in"][0]),
        "ev_w_out": f(I["ev_w_out"][0]),
        "od_w_in": f(I["od_w_in"][0]),
        "od_w_out": f(I["od_w_out"][0]),
        "moe_w_rt": f(np.concatenate([np.asarray(I["moe_w_group"]), np.asarray(I["moe_w_router"])], axis=-1)),
        "moe_w_gate": f(np.asarray(I["moe_w_gate"]).reshape(2, 16, 1024, 256)),
        "moe_w_up": f(np.asarray(I["moe_w_up"]).reshape(2, 16, 1024, 256)),
        "moe_w_down": f(np.asarray(I["moe_w_down"]).reshape(2, 16, 256, 1024)),
        "ple_w_proj": f(I["ple_w_proj"]),
        "ple_w_gate": f(I["ple_w_gate"]),
        "cst": make_cst({kk: np.asarray(v) for kk, v in I.items()}),
        "blkind": (np.arange(SEQ)[None, :] // 256 == np.arange(8)[:, None]).astype(np.float32),
    }
    x = np.asarray(I["x"], dtype=np.float32).reshape(NCORES, TOK, 1024)
    p = np.asarray(I["p"], dtype=np.float32).reshape(2, NCORES, TOK, 256)
    maps = []
    for c in range(NCORES):
        m = dict(shared)
        m["x"] = np.ascontiguousarray(x[c])
        m["p"] = np.ascontiguousarray(p[:, c])
        maps.append(m)
    return maps


def run_stage(I, stage="full", outname="out", trace=False):
    if stage not in _BUILD_CACHE:
        _BUILD_CACHE[stage] = build(stage)
    kb = _BUILD_CACHE[stage]
    maps = prep_inputs(I)
    res = run_bass_kernel_spmd(kb.nc, maps, core_ids=list(range(NCORES)))
    return np.stack([np.asarray(r[outname]) for r in res.results], axis=0)


FUSED = True


def kernel(**inputs):
    if FUSED:
        o = run_stage(inputs, "full", "out")
    else:
        xb = run_stage(inputs, "L0", "xb")
        inputs2 = dict(inputs)
        inputs2["x"] = xb.reshape(16, SEQ, 1024)
        o = run_stage(inputs2, "L1", "out")
    return o.reshape(16, SEQ, 1024).astype(np.float32)
```

```python
import math
import numpy as np
from contextlib import ExitStack
import concourse.bass as bass
import concourse.mybir as mybir
from concourse.bass_utils import run_bass_kernel_spmd

dt = mybir.dt
F32 = dt.float32
BF16 = dt.bfloat16
AF = mybir.ActivationFunctionType
ALU = mybir.AluOpType
AX = mybir.AxisListType

NCORES = 8
SAME_ENGINE_SYNC = True
DBG = {"A", "B", "C", "D", "D1", "D2", "E", "F"}
TOK = 4096
SEQ = 2048
NTS = 16
EPS = 1e-6
EVC = 4112
ODC = 3080


class Unit:
    __slots__ = ("w", "r")

    def __init__(self):
        self.w = None
        self.r = {}


class V:
    __slots__ = ("ap", "units")

    def __init__(self, ap, units):
        self.ap = ap
        self.units = units

    def __getitem__(self, idx):
        return V(self.ap[idx], self.units)

    def rr(self, s, **kw):
        return V(self.ap.rearrange(s, **kw), self.units)

    def bc(self, shape):
        return V(self.ap.to_broadcast(list(shape)), self.units)

    def ub(self, axis, n):
        a = self.ap.unsqueeze(axis)
        shp = list(a.shape)
        shp[axis] = n
        return V(a.to_broadcast(shp), self.units)

    def bitcast(self, d):
        return V(self.ap.bitcast(d), self.units)

    @property
    def shape(self):
        return self.ap.shape


class T:
    def __init__(self, ap, nparts=1):
        self.ap = ap
        self.us = [Unit() for _ in range(nparts)]

    def __getitem__(self, idx):
        return V(self.ap[idx], self.us)

    def u(self, i):
        if isinstance(i, int):
            return V(self.ap, [self.us[i]])
        return V(self.ap, [self.us[j] for j in i])

    def all(self):
        return V(self.ap, self.us)


class K:
    def __init__(self, dma_ring=8):
        self.nc = bass.Bass("TRN2", target_bir_lowering=False)
        self.es = ExitStack()
        nc = self.nc
        self.eng = {"pe": nc.tensor, "dve": nc.vector, "act": nc.scalar,
                    "pool": nc.gpsimd, "sp": nc.sync}
        self.sem = {}
        self.cnt = {}
        self.cur = {}
        self.epoch = {}
        for e in ("pe", "dve", "act", "pool"):
            self.epoch[e] = 0
            self._new_epoch(e)
        self.ring = {}
        self.ringpos = {}
        for q in ("sp", "pool"):
            self.ring[q] = []
            self.ringpos[q] = 0
            for i in range(dma_ring):
                sm = self.es.enter_context(nc.semaphore(f"d_{q}{i}"))
                self.ring[q].append(sm)
                self.sem[("d", q, i)] = sm
                self.cnt[("d", q, i)] = 0
        self.seen = {e: {} for e in ("pe", "dve", "act", "pool", "sp")}
        self.ninst = 0
        self.nwait = 0
        self.scopes = []
        self.uid = 0

    SEM_LIMIT = 30000

    def _new_epoch(self, e):
        key = ("c", e, self.epoch[e])
        self.epoch[e] += 1
        self.sem[key] = self.es.enter_context(self.nc.semaphore(f"s_{e}{key[2]}"))
        self.cnt[key] = 0
        self.cur[e] = key

    def dram(self, name, shape, dtype, kind="Internal", nparts=1):
        t = self.nc.dram_tensor(name, list(shape), dtype, kind=kind)
        return T(t.ap(), nparts)

    def _alloc(self, fn, name, shape, dtype, nparts):
        st = self.scopes[-1] if self.scopes else self.es
        self.uid += 1
        t = st.enter_context(fn(f"{name}_{self.uid}", list(shape), dtype))
        return T(t.ap(), nparts)

    def sb(self, name, shape, dtype=F32, nparts=1):
        return self._alloc(self.nc.sbuf_tensor, name, shape, dtype, nparts)

    def ps(self, name, shape, dtype=F32, nparts=1):
        return self._alloc(self.nc.psum_tensor, name, shape, dtype, nparts)

    def _need(self, e, ev):
        if ev is None:
            return
        key, val = ev
        if key[0] == "c" and key[1] == e and (e == "pe" or not SAME_ENGINE_SYNC):
            return
        if self.seen[e].get(key, 0) >= val:
            return
        self.eng[e].wait_ge(self.sem[key], val)
        self.nwait += 1
        self.seen[e][key] = val

    def _deps(self, e, reads, writes):
        for v in reads:
            for u in v.units:
                self._need(e, u.w)
        for v in writes:
            for u in v.units:
                self._need(e, u.w)
                for k2, val in u.r.items():
                    self._need(e, (k2, val))

    def _mark(self, ev, reads, writes):
        key, val = ev
        for v in reads:
            for u in v.units:
                if u.r.get(key, 0) < val:
                    u.r[key] = val
        for v in writes:
            for u in v.units:
                u.w = ev
                u.r = {}

    def op(self, e, fn, reads, writes, *args, **kw):
        self._deps(e, reads, writes)
        ins = fn(*args, **kw)
        key = self.cur[e]
        self.cnt[key] += 1
        ins.then_inc(self.sem[key], 1)
        self._mark((key, self.cnt[key]), reads, writes)
        self.ninst += 1
        if self.cnt[key] >= self.SEM_LIMIT:
            self._new_epoch(e)
        return ins

    def dma(self, out, in_, q="sp", **kw):
        ring = self.ring[q]
        i = self.ringpos[q] % len(ring)
        self.ringpos[q] += 1
        key = ("d", q, i)
        if self.cnt[key] > 0:
            self._need(q, (key, self.cnt[key]))
        self._deps(q, [in_], [out])
        ins = self.eng[q].dma_start(out=out.ap, in_=in_.ap, **kw)
        self.cnt[key] += 16
        ins.then_inc(self.sem[key], 16)
        self._mark((key, self.cnt[key]), [in_], [out])
        self.ninst += 1
        return ins

    def mm(self, out, lhsT, rhs, start=True, stop=True):
        return self.op("pe", self.nc.tensor.matmul, [lhsT, rhs], [out],
                       out.ap, lhsT.ap, rhs.ap, start=start, stop=stop)

    def tr(self, out, in_, ident):
        return self.op("pe", self.nc.tensor.transpose, [in_, ident], [out],
                       out.ap, in_.ap, ident.ap)

    def act(self, out, in_, func, bias=None, scale=None, accum_out=None):
        reads = [in_]
        writes = [out]
        kw = {}
        if bias is not None:
            if isinstance(bias, V):
                reads.append(bias)
                kw["bias"] = bias.ap
            else:
                kw["bias"] = bias
        if scale is not None:
            if isinstance(scale, V):
                reads.append(scale)
                kw["scale"] = scale.ap
            else:
                kw["scale"] = scale
        if accum_out is not None:
            writes.append(accum_out)
            kw["accum_out"] = accum_out.ap
        return self.op("act", self.nc.scalar.activation, reads, writes,
                       out.ap, in_.ap, func, **kw)

    def tt(self, out, in0, in1, op, e="dve"):
        return self.op(e, self.eng[e].tensor_tensor, [in0, in1], [out],
                       out.ap, in0.ap, in1.ap, op)

    def ts(self, out, in0, s1, s2, op0, op1=None, e="dve", accum_out=None):
        reads = [in0]
        writes = [out]
        a1, a2 = s1, s2
        if isinstance(s1, V):
            reads.append(s1)
            a1 = s1.ap
        if isinstance(s2, V):
            reads.append(s2)
            a2 = s2.ap
        kw = {}
        if op1 is not None:
            kw["op1"] = op1
        if accum_out is not None:
            writes.append(accum_out)
            kw["accum_out"] = accum_out.ap
        return self.op(e, self.eng[e].tensor_scalar, reads, writes,
                       out.ap, in0.ap, a1, a2, op0, **kw)

    def stt(self, out, in0, scalar, in1, op0, op1):
        reads = [in0, in1]
        a = scalar
        if isinstance(scalar, V):
            reads.append(scalar)
            a = scalar.ap
        return self.op("dve", self.nc.vector.scalar_tensor_tensor, reads, [out],
                       out.ap, in0.ap, a, in1.ap, op0, op1)

    def copy(self, out, in_, e="dve"):
        if e == "act":
            return self.op("act", self.nc.scalar.copy, [in_], [out], out.ap, in_.ap)
        return self.op(e, self.eng[e].tensor_copy, [in_], [out], out.ap, in_.ap)

    def memset(self, out, val, e="dve"):
        return self.op(e, self.eng[e].memset, [], [out], out.ap, val)

    def reduce(self, out, in_, op, axis=AX.X):
        return self.op("dve", self.nc.vector.tensor_reduce, [in_], [out],
                       out.ap, in_.ap, axis, op)

    def recip(self, out, in_):
        return self.op("dve", self.nc.vector.reciprocal, [in_], [out], out.ap, in_.ap)

    def max8(self, out, in_):
        return self.op("dve", self.nc.vector.max, [in_], [out], out.ap, in_.ap)

    def barrier(self):
        evs = [(key, c) for key, c in self.cnt.items() if c > 0]
        for e in ("pe", "dve", "act", "pool", "sp"):
            for key, val in evs:
                if self.seen[e].get(key, 0) >= val:
                    continue
                self.eng[e].wait_ge(self.sem[key], val)
                self.nwait += 1
                self.seen[e][key] = val

    def scope(self):
        k = self

        class _S:
            def __enter__(s):
                k.scopes.append(ExitStack())

            def __exit__(s, *a):
                k.barrier()
                k.scopes.pop().close()
                return False
        return _S()

    def finish(self):
        self.barrier()
        self.es.close()


_CL = [("ident", 128), ("tri", 128), ("ones", 128), ("negmask", 128)]
for _l in range(2):
    _CL += [(f"n1g{_l}", 1024), (f"n2g{_l}", 1024), (f"pgg{_l}", 1024), (f"pog{_l}", 1024), (f"rb{_l}", 20)]
_CL += [("fbf", 8), ("fgqk", 1024), ("mcw", 4096), ("mcb", 1024), ("mbi", 4), ("mbf", 4), ("mng", 512)]
_CL += [("ogqk", 1024), ("scw", 4096), ("scb", 1024), ("sdtb", 8), ("salog", 8), ("sD", 8), ("sng", 512)]
COFF = {}
_o = 0
for _n, _w in _CL:
    COFF[_n] = (_o, _w)
    _o += _w
NCST = _o


def make_cst(I):
    c = np.zeros((128, NCST), np.float32)

    def put(name, row):
        o, w = COFF[name]
        row = np.asarray(row, np.float32).reshape(-1)
        assert row.shape[0] == w, (name, row.shape, w)
        c[:, o:o + w] = row[None, :]

    o, w = COFF["ident"]
    c[:, o:o + w] = np.eye(128, dtype=np.float32)
    s = np.arange(128)[:, None]
    t = np.arange(128)[None, :]
    o, w = COFF["tri"]
    c[:, o:o + w] = (s <= t).astype(np.float32)
    o, w = COFF["ones"]
    c[:, o:o + w] = 1.0
    o, w = COFF["negmask"]
    c[:, o:o + w] = np.where(s <= t, 0.0, -1.0e4).astype(np.float32)
    for l in range(2):
        put(f"n1g{l}", I["norm1_g"][l])
        put(f"n2g{l}", I["norm2_g"][l])
        put(f"pgg{l}", I["ple_gate_norm_g"][l])
        put(f"pog{l}", I["ple_out_norm_g"][l])
        put(f"rb{l}", np.concatenate([I["moe_b_group"][l], I["moe_b_router"][l]]))
    put("fbf", I["ev_fox_b_f"][0])
    put("fgqk", np.concatenate([np.tile(I["ev_fox_qn_g"][0], 8), np.tile(I["ev_fox_kn_g"][0], 8)]))
    put("mcw", I["ev_mlstm_conv_w"][0].reshape(-1))
    put("mcb", I["ev_mlstm_conv_b"][0])
    put("mbi", I["ev_mlstm_b_i"][0])
    put("mbf", I["ev_mlstm_b_f"][0])
    put("mng", I["ev_mlstm_norm_g"][0])
    put("ogqk", np.concatenate([np.tile(I["od_moba_qn_g"][0], 8), np.tile(I["od_moba_kn_g"][0], 8)]))
    put("scw", I["od_ssd_conv_w"][0].reshape(-1))
    put("scb", I["od_ssd_conv_b"][0])
    put("sdtb", I["od_ssd_dt_bias"][0])
    put("salog", I["od_ssd_A_log"][0])
    put("sD", I["od_ssd_D"][0])
    put("sng", I["od_ssd_norm_g"][0])
    return c


def build(stage="full"):
    k = K()
    nc = k.nc
    EI = "ExternalInput"
    x_in = k.dram("x", [TOK, 1024], F32, EI, nparts=32)
    p_in = k.dram("p", [2, TOK, 256], F32, EI)
    ev_w_in = k.dram("ev_w_in", [1024, EVC], F32, EI)
    ev_w_out = k.dram("ev_w_out", [1024, 1024], F32, EI)
    od_w_in = k.dram("od_w_in", [1024, ODC], F32, EI)
    od_w_out = k.dram("od_w_out", [1024, 1024], F32, EI)
    moe_w_rt = k.dram("moe_w_rt", [2, 1024, 20], F32, EI)
    moe_w_gate = k.dram("moe_w_gate", [2, 16, 1024, 256], F32, EI)
    moe_w_up = k.dram("moe_w_up", [2, 16, 1024, 256], F32, EI)
    moe_w_down = k.dram("moe_w_down", [2, 16, 256, 1024], F32, EI)
    ple_w_proj = k.dram("ple_w_proj", [2, 256, 1024], F32, EI)
    ple_w_gate = k.dram("ple_w_gate", [2, 1024, 1024], F32, EI)
    cst = k.dram("cst", [128, NCST], F32, EI)
    blkind = k.dram("blkind", [8, SEQ], F32, EI)

    EO = "ExternalOutput"
    MIX = k.dram("mix", [TOK, 1024], F32, EO if stage in ("L0mix", "L1mix") else "Internal", nparts=32)
    XB = k.dram("xb", [TOK, 1024], F32, EO if stage in ("L0", "POST") else "Internal", nparts=32)
    OUT = k.dram("out", [TOK, 1024], F32, EO, nparts=32)
    GF = k.dram("gf", [TOK, 512], F32, nparts=32)
    UM = k.dram("um", [2, 3 + SEQ, 2056], F32, nparts=34)

    def crow(name, c0=0, c1=None, dtype=F32):
        o, w = COFF[name]
        if c1 is None:
            c1 = w
        t = k.sb("c_" + name, [128, c1 - c0], F32)
        k.dma(t[:], cst[:, o + c0:o + c1])
        return t

    ident_f = crow("ident")
    tri_f = crow("tri")
    ones_f = crow("ones")
    ident_b = k.sb("ident_b", [128, 128], BF16)
    tri_b = k.sb("tri_b", [128, 128], BF16)
    ones_b = k.sb("ones_b", [128, 128], BF16)
    k.copy(ident_b[:], ident_f[:])
    k.copy(tri_b[:], tri_f[:])
    k.copy(ones_b[:], ones_f[:])
    P = [k.ps(f"P{i}", [128, 512], F32, nparts=1) for i in range(8)]

    def pq(b, q):
        return P[b].all()[:, q * 128:(q + 1) * 128]

    def pbf(b):
        return P[b].all().bitcast(BF16)

    ctr = {"cast": 0}

    def cast_rr(out, in_):
        e = ("pool", "dve", "act")[ctr["cast"] % 3]
        ctr["cast"] += 1
        k.copy(out, in_, e=e)

    def load_w_bf16(dst, src_ap_fn, nkc, ncols, stage_tiles):
        for kc in range(nkc):
            st = stage_tiles[kc % len(stage_tiles)]
            k.dma(st[:, 0:ncols], src_ap_fn(kc))
            cast_rr(dst[:, kc, :], st[:, 0:ncols])

    def act_rstd(v, n):
        k.act(v, v, AF.Ln, bias=EPS, scale=1.0 / n)
        k.act(v, v, AF.Exp, scale=-0.5)

    def act_sigmoid(out, in_):
        k.act(out, in_, AF.Exp, scale=-1.0)
        k.act(out, out, AF.Ln, bias=1.0)
        k.act(out, out, AF.Exp, scale=-1.0)

    def rms_rstd(out_rstd, src, n, junk):
        k.act(junk, src, AF.Square, accum_out=out_rstd)
        act_rstd(out_rstd, n)

    def transpose_to(dstT, src_b, nblk, banks, ident, bf=True):
        for g in range((nblk + 3) // 4):
            nb = min(4, nblk - g * 4)
            bank = banks[g % len(banks)]
            pv = pbf(bank) if bf else P[bank].all()
            for q in range(nb):
                b = g * 4 + q
                k.tr(pv[:, q * 128:(q + 1) * 128], src_b[:, b * 128:(b + 1) * 128], ident[:])
            k.copy(dstT[:, g * 4:g * 4 + nb, :],
                   pv[:, 0:nb * 128].rr("p (a b) -> p a b", a=nb),
                   e=("act" if g % 2 else "dve"))

    def phaseA_pipeline(front, proj_units, post_units):
        front(0)
        for u_ in proj_units(0):
            u_()
        for i in range(NTS):
            if i + 1 < NTS:
                front(i + 1)
                pu = proj_units(i + 1)
            else:
                pu = []
            po = post_units(i)
            n_po, n_pu = len(po), len(pu)
            done_pu = 0
            for a, p_ in enumerate(po):
                p_()
                want = ((a + 1) * n_pu) // n_po
                while done_pu < want:
                    pu[done_pu]()
                    done_pu += 1
            while done_pu < n_pu:
                pu[done_pu]()
                done_pu += 1

    def run_interleaved(ga, gb, ratio):
        a_done = b_done = False
        while not (a_done and b_done):
            for _ in range(ratio):
                if a_done:
                    break
                try:
                    next(ga)
                except StopIteration:
                    a_done = True
            if not b_done:
                try:
                    next(gb)
                except StopIteration:
                    b_done = True

    def attention(qT_of, kT_of, Vaug, bias_of, emit_out, banksS, banksO):
        NG = len(banksS)
        PT = [[k.sb(f"PT{g}_{q}", [128, 128], BF16) for q in range(4)] for g in range(NG)]
        rden = k.sb("rden", [128, 8, 1])
        oas = [k.sb(f"oa{n}", [128, 512]) for n in range(2)]
        for i in range(NTS):
            oa = oas[i % 2]
            items = [(h, j) for h in range(8) for j in range(i + 1)]
            groups = [items[a:a + 4] for a in range(0, len(items), 4)]
            pob = banksO[(i % 2) * 2:(i % 2) * 2 + 2]
            biasfn = bias_of(i)

            def po(h):
                return P[pob[h // 4]].all()[:, (h % 4) * 65:(h % 4) * 65 + 65]

            def stage1(g):
                bank = banksS[g % NG]
                for q, (h, j) in enumerate(groups[g]):
                    k.mm(pq(bank, q), kT_of(h, j), qT_of(h, i))
                for q, (h, j) in enumerate(groups[g]):
                    pt = PT[g % NG][q]
                    bv = biasfn(h, j) if biasfn is not None else None
                    k.act(pt[:], pq(bank, q), AF.Exp, bias=bv, scale=0.125)
                    if j == i:
                        k.tt(pt[:], pt[:], tri_b[:], ALU.mult)

            def stage2(g):
                for q, (h, j) in enumerate(groups[g]):
                    k.mm(po(h), PT[g % NG][q][:], Vaug.u(j)[:, j, h * 65:(h + 1) * 65],
                         start=(j == 0), stop=(j == i))
                    if j == i and h % 4 == 3:
                        hb = h // 4
                        pb = P[pob[hb]].all()[:, 0:260].rr("p (h e) -> p h e", e=65)
                        k.recip(rden[:, hb * 4:(hb + 1) * 4, :], pb[:, :, 64:65])
                        k.tt(oa[:, hb * 256:(hb + 1) * 256].rr("p (h d) -> p h d", d=64), pb[:, :, 0:64],
                             rden[:, hb * 4:(hb + 1) * 4, :].bc([128, 4, 64]), ALU.mult)

            LOOK = NG - 1
            N = len(groups)
            for g in range(min(LOOK, N)):
                stage1(g)
            for g in range(N):
                if g + LOOK < N:
                    stage1(g + LOOK)
                stage2(g)
                yield
            emit_out(i, oa)
            yield

    def qk_headnorm(u, sq, ssqk, qkb, gqk):
        k.act(sq[:], u[:, 0:1024], AF.Square)
        k.reduce(ssqk[:], sq[:].rr("p (h d) -> p h d", d=64), ALU.add)
        act_rstd(ssqk[:], 64)
        k.tt(sq[:].rr("p (h d) -> p h d", d=64), u[:, 0:1024].rr("p (h d) -> p h d", d=64),
             ssqk[:].ub(2, 64), ALU.mult)
        k.tt(qkb[:], sq[:], gqk[:], ALU.mult)

    def logsig(out, in_, tmp):
        k.act(tmp, in_, AF.Exp, scale=-1.0)
        k.act(tmp, tmp, AF.Ln, bias=1.0)
        k.ts(out, tmp, -1.0, None, ALU.mult)

    def conv_part1(acc, ws, w3v, cw, cb, t1s):
        srcs = [ws[0][:], ws[1][:], ws[2][:], w3v]
        for j in range(4):
            k.tt(t1s[j][:], srcs[j], cw[:, j * 1024:(j + 1) * 1024], ALU.mult, e="pool")
        k.tt(acc[:], t1s[0][:], t1s[1][:], ALU.add, e="pool")
        k.tt(t1s[2][:], t1s[2][:], t1s[3][:], ALU.add, e="pool")
        k.tt(acc[:], acc[:], t1s[2][:], ALU.add, e="pool")
        k.tt(acc[:], acc[:], cb[:], ALU.add, e="pool")

    def conv_part2(dst, acc, sig):
        act_sigmoid(sig[:], acc[:])
        k.tt(dst, acc[:], sig[:], ALU.mult)

    def zero_um_pad():
        z = k.sb("zpad", [128, 2056])
        k.memset(z[:], 0.0)
        for s in range(2):
            k.dma(UM.u(s * 17)[s, 0:3, :], z[0:3, :], q="pool")

    def layer0_mixers(X):
        with k.scope():
            zero_um_pad()
        for s in range(2):
            with k.scope():
                qT = k.sb("qT", [64, 8, SEQ], BF16, nparts=NTS)
                kT = k.sb("kT", [64, 8, SEQ], BF16, nparts=NTS)
                Vaug = k.sb("Vaug", [128, NTS, 520], BF16, nparts=NTS)
                CF = k.sb("CF", [128, NTS, 8])
                CFE = k.sb("CFE", [128, NTS, 8])
                k.memset(Vaug.all()[:, :, :], 1.0)
                with k.scope():
                    wb = k.sb("wb_in", [128, 8, EVC], BF16)
                    with k.scope():
                        stg = [k.sb(f"stg{n}", [128, EVC]) for n in range(2)]
                        load_w_bf16(wb, lambda kc: ev_w_in[kc * 128:(kc + 1) * 128, :], 8, EVC, stg)
                    n1g = crow("n1g0")
                    gqk = crow("fgqk")
                    fbf = crow("fbf")
                    xts = [k.sb(f"xt{n}", [128, 1024]) for n in range(1)]
                    junk = k.sb("junk", [128, 1024])
                    hbs = [k.sb(f"hb{n}", [128, 1024], BF16) for n in range(2)]
                    hTs = [k.sb(f"hT{n}", [128, 8, 128], BF16) for n in range(2)]
                    UA = 2560
                    uas = [k.sb(f"ua{n}", [128, UA]) for n in range(2)]
                    ub_ = k.sb("ub", [128, EVC - UA])
                    rss = [k.sb(f"rs{n}", [128, 1]) for n in range(2)]
                    ssqk = k.sb("ssqk", [128, 16])
                    qkb = k.sb("qkb", [128, 1024], BF16)
                    t8 = k.sb("t8", [128, 8])
                    lf = k.sb("lf", [128, 8])
                    LFacc = k.sb("LFacc", [128, 8])
                    gt = k.sb("gt", [128, 512])

                    def U(i, c0, c1):
                        if c1 <= UA:
                            return uas[i % 2][:, c0:c1]
                        assert c0 >= UA
                        return ub_[:, c0 - UA:c1 - UA]

                    def front(i):
                        ti = s * NTS + i
                        r0 = ti * 128
                        xt, hb, hT, rs = xts[0], hbs[i % 2], hTs[i % 2], rss[i % 2]
                        k.dma(xt[:], X.u(ti)[r0:r0 + 128, :])
                        rms_rstd(rs[:], xt[:], 1024, junk[:])
                        k.stt(hb[:], xt[:], rs[:], n1g[:], ALU.mult, ALU.mult)
                        transpose_to(hT, hb, 8, [0, 1], ident_b)

                    def proj_units(i):
                        hT = hTs[i % 2]

                        def chunk(c):
                            c0 = c * 512
                            cw_ = min(512, EVC - c0)
                            pb = P[2 + c % 4].all()
                            for kc in range(8):
                                k.mm(pb[:, 0:cw_], hT[:, kc, :], wb[:, kc, c0:c0 + cw_],
                                     start=(kc == 0), stop=(kc == 7))
                            k.copy(U(i, c0, c0 + cw_), pb[:, 0:cw_], e=("act" if c % 2 else "dve"))
                        return [(lambda c=c: chunk(c)) for c in range(9)]

                    def post_units(i):
                        ti = s * NTS + i
                        r0 = ti * 128
                        uq = U(i, 0, 1024)

                        def p_um():
                            k.dma(UM.u(s * 17 + 1 + i)[s, 3 + i * 128:3 + (i + 1) * 128, UA - 2056:2056],
                                  U(i, UA, EVC), q="pool")
                            k.dma(UM.u(s * 17 + 1 + i)[s, 3 + i * 128:3 + (i + 1) * 128, 0:UA - 2056],
                                  U(i, 2056, UA), q="pool")

                        def p_norm1():
                            k.act(junk[:], uq, AF.Square)
                            k.reduce(ssqk[:], junk[:].rr("p (h d) -> p h d", d=64), ALU.add)
                            act_rstd(ssqk[:], 64)

                        def p_norm2():
                            k.tt(junk[:].rr("p (h d) -> p h d", d=64), uq.rr("p (h d) -> p h d", d=64),
                                 ssqk[:].ub(2, 64), ALU.mult)
                            k.tt(qkb[:], junk[:], gqk[:], ALU.mult)

                        def p_tr(half):
                            dst = (qT, kT)[half]
                            pv = pbf(half)
                            for h in range(8):
                                c0 = half * 512 + h * 64
                                k.tr(pv[0:64, h * 128:(h + 1) * 128], qkb[:, c0:c0 + 64], ident_b[:])
                            k.copy(dst.u(i)[0:64, :, i * 128:(i + 1) * 128],
                                   pv[0:64, 0:1024].rr("p (a b) -> p a b", a=8),
                                   e=("act" if half else "dve"))

                        def p_v():
                            k.copy(Vaug.u(i)[:, i, :].rr("p (h e) -> p h e", e=65)[:, :, 0:64],
                                   U(i, 1024, 1536).rr("p (h d) -> p h d", d=64), e="pool")

                        def p_lf():
                            k.tt(t8[:], U(i, 1536, 1544), fbf[:], ALU.add)
                            logsig(lf[:], t8[:], t8[:])

                        def p_cf():
                            pc = P[6].all()
                            k.mm(pc[:, 0:8], tri_f[:], lf[:], start=True, stop=(i == 0))
                            if i > 0:
                                k.mm(pc[:, 0:8], ones_f[:], LFacc[:], start=False, stop=True)
                            k.copy(CF[:, i, :], pc[:, 0:8])
                            if i == 0:
                                k.copy(LFacc[:], lf[:])
                            else:
                                k.tt(LFacc[:], LFacc[:], lf[:], ALU.add)
                            k.mm(pc[:, 8:16], ones_f[:], LFacc[:], start=True, stop=True)
                            k.copy(CFE[:, i, :], pc[:, 8:16])

                        def p_gate():
                            act_sigmoid(gt[:], U(i, 1544, 2056))
                            k.dma(GF.u(ti)[r0:r0 + 128, :], gt[:], q="pool")

                        return [p_um, p_norm1, p_norm2, lambda: p_tr(0), lambda: p_tr(1), p_v, p_lf, p_cf, p_gate]

                    phaseA_pipeline(front, proj_units, post_units)
                with k.scope():
                    NBs = [k.sb(f"NB{n}", [128, NTS, 8]) for n in range(2)]
                    gts = [k.sb(f"gts{n}", [128, 512]) for n in range(2)]

                    def bias_of(i):
                        NB = NBs[i % 2]
                        k.tt(NB[:, 0:i + 1, :], CFE[:, i:i + 1, :].bc([128, i + 1, 8]),
                             CF[:, 0:i + 1, :], ALU.subtract)
                        return lambda h, j: NB[:, j, h:h + 1]

                    def emit(i, oa):
                        ti = s * NTS + i
                        r0 = ti * 128
                        g = gts[i % 2]
                        k.dma(g[:], GF.u(ti)[r0:r0 + 128, :])
                        k.tt(oa[:], oa[:], g[:], ALU.mult)
                        k.dma(MIX.u(ti)[r0:r0 + 128, 0:512], oa[:], q="pool")

                    genB = attention(lambda h, i: qT.u(i)[0:64, h, i * 128:(i + 1) * 128],
                                     lambda h, j: kT.u(j)[0:64, h, j * 128:(j + 1) * 128],
                                     Vaug, bias_of, emit, [0, 1], [2, 3, 2, 3])
                    genC = mlstm_phase(s)
                    run_interleaved(genB, genC, 2)

    def mlstm_phase(s):
        cw = crow("mcw")
        cb = crow("mcb")
        mbi = crow("mbi")
        mbf = crow("mbf")
        mng = crow("mng")
        w3s = [k.sb(f"w3{n}", [128, 2056]) for n in range(2)]
        wss = [[k.sb(f"ws{n}_{j}", [128, 1024]) for j in range(3)] for n in range(2)]
        accs = [k.sb(f"cacc{n}", [128, 1024]) for n in range(2)]
        t1s = [k.sb(f"ct1_{n}", [128, 1024]) for n in range(4)]
        csig = k.sb("csig", [128, 1024])
        qk = k.sb("mqk", [128, 1024])
        g8 = k.sb("g8", [128, 4])
        lfm = k.sb("lfm", [128, 4])
        ic = k.sb("ic", [128, 4])
        qs = k.sb("qs", [128, 4])
        ks = k.sb("ks", [128, 4])
        ebl = k.sb("ebl", [128, 4])
        qsb = k.sb("qsb", [128, 4, 128], BF16)
        ksb = k.sb("ksb", [128, 4, 128], BF16)
        qTt = k.sb("qTt", [128, 4, 128], BF16)
        kTt = k.sb("kTt", [128, 4, 128], BF16)
        va = k.sb("va", [128, 4, 129], BF16)
        wTs = [k.sb(f"wT{n}", [128, 128], BF16) for n in range(2)]
        Uf = k.sb("Uf", [128, 4, 129], nparts=4)
        Ub = k.sb("Ub", [128, 4, 129], BF16, nparts=4)
        den = k.sb("mden", [128, 4])
        hc = k.sb("hc", [128, 4, 128])
        sqh = k.sb("sqh", [128, 512])
        ssh = k.sb("ssh", [128, 4])
        sg = k.sb("msg", [128, 512])
        k.memset(Uf.all()[:, :, :], 0.0)
        k.memset(Ub.all()[:, :, :], 0.0)
        k.memset(va[:, :, 128:129], 1.0)

        def front(i):
            r = 3 + i * 128
            uu = s * 17 + 1 + i
            k.dma(w3s[i % 2][:], UM.u(uu)[s, r:r + 128, :])
            for j in range(3):
                k.dma(wss[i % 2][j][:], UM.u([uu - 1, uu])[s, r - 3 + j:r - 3 + j + 128, 0:1024])
            conv_part1(accs[i % 2], wss[i % 2], w3s[i % 2][:, 0:1024], cw, cb, t1s)

        front(0)
        for i in range(NTS):
            ti = s * NTS + i
            r0 = ti * 128
            w3 = w3s[i % 2]
            if i + 1 < NTS:
                front(i + 1)
            yield
            conv_part2(qk[:], accs[i % 2], csig)
            yield
            k.tt(g8[:], w3[:, 1540:1544], mbf[:], ALU.add)
            logsig(lfm[:], g8[:], g8[:])
            k.tt(ic[:], w3[:, 1536:1540], mbi[:], ALU.add)
            pc = P[7].all()
            k.mm(pc[:, 0:4], tri_f[:], lfm[:])
            k.mm(pc[:, 4:8], ones_f[:], lfm[:])
            k.act(qs[:], pc[:, 0:4], AF.Exp)
            k.tt(g8[:], ic[:], pc[:, 0:4], ALU.subtract)
            k.act(ks[:], g8[:], AF.Exp, bias=-0.5 * math.log(128.0))
            k.act(ebl[:], pc[:, 4:8], AF.Exp)
            yield
            k.tt(qsb[:], qk[:, 0:512].rr("p (h d) -> p h d", d=128), qs[:].ub(2, 128), ALU.mult)
            k.tt(ksb[:], qk[:, 512:1024].rr("p (h d) -> p h d", d=128), ks[:].ub(2, 128), ALU.mult)
            transpose_to(qTt, qsb[:].rr("p h d -> p (h d)"), 4, [4], ident_b)
            transpose_to(kTt, ksb[:].rr("p h d -> p (h d)"), 4, [4], ident_b)
            k.copy(va[:, :, 0:128], w3[:, 1024:1536].rr("p (h d) -> p h d", d=128), e="pool")
            yield
            for h in range(4):
                pss = pq(5, 0)
                k.mm(pss, kTt[:, h, :], qTt[:, h, :])
                wT = wTs[h % 2]
                k.tt(wT[:], pss, tri_f[:], ALU.mult)
                pn = P[6].all()[:, 0:129]
                k.mm(pn, wT[:], va[:, h, :], start=True, stop=False)
                k.mm(pn, qTt[:, h, :], Ub.u(h)[:, h, :], start=False, stop=True)
                pu = P[7].all()[:, 0:129]
                k.mm(pu, ksb[:, h, :], va[:, h, :])
                k.tt(Uf.u(h)[:, h, :], Uf.u(h)[:, h, :], pu, ALU.add)
                k.ts(Uf.u(h)[:, h, :], Uf.u(h)[:, h, :], ebl[:, h:h + 1], None, ALU.mult)
                k.copy(Ub.u(h)[:, h, :], Uf.u(h)[:, h, :], e="pool")
                k.act(den[:, h:h + 1], pn[:, 128:129], AF.Abs)
                k.ts(den[:, h:h + 1], den[:, h:h + 1], 1.0, None, ALU.max)
                k.recip(den[:, h:h + 1], den[:, h:h + 1])
                k.ts(hc[:, h, :], pn[:, 0:128], den[:, h:h + 1], None, ALU.mult)
                yield
            hcf = hc[:].rr("p h d -> p (h d)")
            k.act(sqh[:], hcf, AF.Square)
            k.reduce(ssh[:], sqh[:].rr("p (h d) -> p h d", d=128), ALU.add)
            act_rstd(ssh[:], 128)
            k.tt(hc[:], hc[:], ssh[:].ub(2, 128), ALU.mult)
            k.tt(hcf, hcf, mng[:], ALU.mult)
            act_sigmoid(sg[:], w3[:, 1544:2056])
            k.tt(sqh[:], hcf, sg[:], ALU.mult)
            k.dma(MIX.u(ti)[r0:r0 + 128, 512:1024], sqh[:], q="pool")
            yield

    def layer1_mixers(X):
        with k.scope():
            zero_um_pad()
        for s in range(2):
            with k.scope():
                qTa = k.sb("qTa", [72, 8, SEQ], BF16, nparts=NTS)
                kTa = k.sb("kTa", [72, 8, SEQ], BF16, nparts=NTS)
                Vaug = k.sb("Vaug1", [128, NTS, 520], BF16, nparts=NTS)
                k.memset(Vaug.all()[:, :, :], 1.0)
                with k.scope():
                    wb = k.sb("wb_in1", [128, 8, ODC], BF16)
                    with k.scope():
                        stg = [k.sb(f"stg1_{n}", [128, ODC]) for n in range(2)]
                        load_w_bf16(wb, lambda kc: od_w_in[kc * 128:(kc + 1) * 128, :], 8, ODC, stg)
                    with k.scope():
                        bis = k.sb("bis", [72, SEQ])
                        k.dma(bis[64:72, :], blkind[:, :])
                        for h in range(8):
                            k.copy(kTa.all()[64:72, h, :], bis[64:72, :], e=("act" if h % 2 else "dve"))
                    n1g = crow("n1g1")
                    gqk = crow("ogqk")
                    xts = [k.sb(f"xt{n}", [128, 1024]) for n in range(2)]
                    junk = k.sb("junk", [128, 1024])
                    hbs = [k.sb(f"hb{n}", [128, 1024], BF16) for n in range(2)]
                    hTs = [k.sb(f"hT{n}", [128, 8, 128], BF16) for n in range(2)]
                    u = k.sb("u", [128, ODC])
                    rs = k.sb("rs", [128, 1])
                    ssqk = k.sb("ssqk", [128, 16])
                    qkb = k.sb("qkb", [128, 1024], BF16)
                    KS = k.sb("KS", [64, 8, NTS])
                    kmf = k.sb("kmf", [64, 8, 8])
                    kmT = k.sb("kmT", [64, 8, 8], BF16)
                    G = k.sb("G", [128, 8, 8])
                    m8 = k.sb("m8", [128, 8, 8])
                    MB = k.sb("MB", [128, 8, 8])
                    MBb = k.sb("MBb", [128, 64], BF16)
                    MBT = k.sb("MBT", [8, 8, 128], BF16)
                    k.memset(kmT[:], 0.0)
                    for i in range(NTS):
                        ti = s * NTS + i
                        r0 = ti * 128
                        own = i // 2
                        xt, hb, hT = xts[i % 2], hbs[i % 2], hTs[i % 2]
                        k.dma(xt[:], X.u(ti)[r0:r0 + 128, :])
                        rms_rstd(rs[:], xt[:], 1024, junk[:])
                        k.stt(hb[:], xt[:], rs[:], n1g[:], ALU.mult, ALU.mult)
                        transpose_to(hT, hb, 8, [0, 1], ident_b)
                        for c in range(7):
                            c0 = c * 512
                            cw_ = min(512, ODC - c0)
                            pb = P[2 + c % 4].all()
                            for kc in range(8):
                                k.mm(pb[:, 0:cw_], hT[:, kc, :], wb[:, kc, c0:c0 + cw_],
                                     start=(kc == 0), stop=(kc == 7))
                            k.copy(u[:, c0:c0 + cw_], pb[:, 0:cw_], e=("act" if c % 2 else "dve"))
                        qk_headnorm(u, junk, ssqk, qkb, gqk)
                        for half, dst in ((0, qTa), (1, kTa)):
                            pv = pbf(half)
                            for h in range(8):
                                c0 = half * 512 + h * 64
                                k.tr(pv[0:64, h * 128:(h + 1) * 128], qkb[:, c0:c0 + 64], ident_b[:])
                            k.copy(dst.u(i)[0:64, :, i * 128:(i + 1) * 128],
                                   pv[0:64, 0:1024].rr("p (a b) -> p a b", a=8),
                                   e=("act" if half else "dve"))
                        k.copy(Vaug.u(i)[:, i, :].rr("p (h e) -> p h e", e=65)[:, :, 0:64],
                               u[:, 1024:1536].rr("p (h d) -> p h d", d=64), e="pool")
                        pk = P[6].all()
                        for h in range(8):
                            k.mm(pk[0:64, h:h + 1], qkb[:, 512 + h * 64:512 + (h + 1) * 64], ones_b[:, 0:1])
                        k.copy(KS[:, :, i], pk[0:64, 0:8])
                        if own >= 1:
                            pg = P[7].all()
                            for h in range(8):
                                k.mm(pg[:, h * 8:(h + 1) * 8], qTa.u(i)[0:64, h, i * 128:(i + 1) * 128], kmT[:, h, :])
                            k.memset(G[:], -1.0e30)
                            k.copy(G[:, :, 0:own], pg[:, 0:64].rr("p (h n) -> p h n", n=8)[:, :, 0:own])
                            for h in range(8):
                                k.max8(m8[:, h, :], G[:, h, :])
                            k.tt(MB[:], G[:], m8[:, :, 2:3].bc([128, 8, 8]), ALU.is_lt)
                            k.ts(MB[:], MB[:], -4096.0, None, ALU.mult)
                            if own < 8:
                                k.memset(MB[:, :, own:8], 0.0)
                            k.copy(MBb[:], MB[:].rr("p h n -> p (h n)"))
                        else:
                            k.memset(MBb[:], 0.0)
                        pm = pbf(7)
                        for h in range(8):
                            k.tr(pm[0:8, h * 128:(h + 1) * 128], MBb[:, h * 8:(h + 1) * 8], ident_b[:])
                        k.copy(MBT[:], pm[0:8, 0:1024].rr("p (a b) -> p a b", a=8))
                        k.dma(qTa.u(i)[64:72, :, i * 128:(i + 1) * 128], MBT[:], q="pool")
                        if i % 2 == 1:
                            k.tt(kmf[:, :, own], KS[:, :, i - 1], KS[:, :, i], ALU.add)
                            k.ts(kmT[:, :, own], kmf[:, :, own], 1.0 / 256, None, ALU.mult)
                        k.dma(UM.u(s * 17 + 1 + i)[s, 3 + i * 128:3 + (i + 1) * 128, 0:1544],
                              u[:, 1536:ODC], q="pool")
                with k.scope():
                    def emit(i, oa):
                        ti = s * NTS + i
                        r0 = ti * 128
                        k.dma(MIX.u(ti)[r0:r0 + 128, 0:512], oa[:], q="pool")

                    genB = attention(lambda h, i: qTa.u(i)[0:72, h, i * 128:(i + 1) * 128],
                                     lambda h, j: kTa.u(j)[0:72, h, j * 128:(j + 1) * 128],
                                     Vaug, lambda i: None, emit, [0, 1], [2, 3, 2, 3])
                    genC = ssd_phase(s)
                    run_interleaved(genB, genC, 2)

    def ssd_phase(s):
        cw = crow("scw")
        cb = crow("scb")
        dtb = crow("sdtb")
        alog = crow("salog")
        Dsk = crow("sD")
        sng = crow("sng")
        negm = crow("negmask")
        w3s = [k.sb(f"sw3{n}", [128, 1544]) for n in range(2)]
        wss = [[k.sb(f"sws{n}_{j}", [128, 1024]) for j in range(3)] for n in range(2)]
        accs = [k.sb(f"sacc{n}", [128, 1024]) for n in range(2)]
        t1s = [k.sb(f"st1_{n}", [128, 1024]) for n in range(4)]
        csig = k.sb("ssig", [128, 1024])
        xbc = k.sb("xbc", [128, 1024])
        Aex = k.sb("Aex", [128, 8])
        dtt = k.sb("dtt", [128, 8])
        adt = k.sb("adt", [128, 8])
        adtb = k.sb("adtb", [128, 128])
        bcs = k.sb("bcs", [128, 8])
        eb = k.sb("eb", [128, 8])
        ebl = k.sb("sebl", [128, 8])
        dec = k.sb("dec", [128, 8])
        xdt = k.sb("xdt", [128, 8, 64])
        xdtb = k.sb("xdtb", [128, 8, 64], BF16)
        xdd = k.sb("xdd", [128, 8, 64], BF16)
        BCb = k.sb("BCb", [128, 512], BF16)
        BCT = k.sb("BCT", [128, 4, 128], BF16)
        GT = k.sb("GT", [128, 2, 128])
        tmpL = [k.sb(f"tmpL{n}", [128, 128]) for n in range(2)]
        LT = [k.sb(f"LT{n}", [128, 128]) for n in range(2)]
        WT = [k.sb(f"sWT{n}", [128, 128], BF16) for n in range(2)]
        Hf = k.sb("Hf", [128, 8, 64])
        Hb = k.sb("Hb", [128, 8, 64], BF16)
        y1 = k.sb("y1", [128, 512])
        y2 = k.sb("y2", [128, 512])
        sz = k.sb("sz", [128, 512])
        ssg = k.sb("ssg", [128, 2])
        k.memset(Hf[:], 0.0)
        k.memset(Hb[:], 0.0)
        k.act(Aex[:], alog[:], AF.Exp)

        def front(i):
            r = 3 + i * 128
            uu = s * 17 + 1 + i
            k.dma(w3s[i % 2][:], UM.u(uu)[s, r:r + 128, 0:1544])
            for j in range(3):
                k.dma(wss[i % 2][j][:], UM.u([uu - 1, uu])[s, r - 3 + j:r - 3 + j + 128, 512:1536])
            conv_part1(accs[i % 2], wss[i % 2], w3s[i % 2][:, 512:1536], cw, cb, t1s)

        front(0)
        for i in range(NTS):
            ti = s * NTS + i
            r0 = ti * 128
            w3 = w3s[i % 2]
            if i + 1 < NTS:
                front(i + 1)
            yield
            conv_part2(xbc[:], accs[i % 2], csig)
            yield
            k.tt(dtt[:], w3[:, 1536:1544], dtb[:], ALU.add)
            k.act(dtt[:], dtt[:], AF.Exp)
            k.act(dtt[:], dtt[:], AF.Ln, bias=1.0)
            k.tt(adt[:], dtt[:], Aex[:], ALU.mult)
            k.ts(adt[:], adt[:], -1.0, None, ALU.mult)
            pc = P[4].all()
            k.mm(pc[:, 0:8], tri_f[:], adt[:])
            k.mm(pc[:, 8:16], ones_f[:], adt[:])
            k.copy(bcs[:], pc[:, 0:8])
            k.act(eb[:], pc[:, 0:8], AF.Exp)
            k.act(ebl[:], pc[:, 8:16], AF.Exp)
            k.tt(dec[:], pc[:, 8:16], bcs[:], ALU.subtract)
            k.act(dec[:], dec[:], AF.Exp)
            yield
            xs3 = xbc[:, 0:512].rr("p (h d) -> p h d", d=64)
            k.tt(xdt[:], xs3, dtt[:].ub(2, 64), ALU.mult)
            k.copy(xdtb[:], xdt[:], e="pool")
            k.tt(xdd[:], xdt[:], dec[:].ub(2, 64), ALU.mult)
            k.copy(BCb[:], xbc[:, 512:1024], e="pool")
            transpose_to(BCT, BCb, 4, [4], ident_b)
            for g in range(2):
                k.mm(pq(4, g), BCT[:, g, :], BCT[:, 2 + g, :])
            k.copy(GT[:], P[4].all()[:, 0:256].rr("p (g l) -> p g l", g=2))
            yield
            pyd = P[6].all()
            pyo = P[7].all()
            for h in range(8):
                g = h // 4
                k.copy(adtb[:], adt[:, h:h + 1].bc([128, 128]), e="pool")
                pbb = pq(5, 0)
                k.mm(pbb, adtb[:], tri_f[:])
                tl = tmpL[h % 2]
                k.stt(tl[:], pbb, bcs[:, h:h + 1], negm[:], ALU.subtract, ALU.min)
                lt = LT[h % 2]
                k.act(lt[:], tl[:], AF.Exp)
                wt = WT[h % 2]
                k.tt(wt[:], GT[:, g, :], lt[:], ALU.mult)
                k.mm(pyd[:, h * 64:(h + 1) * 64], wt[:], xdtb[:, h, :])
                k.mm(pyo[:, h * 64:(h + 1) * 64], BCT[:, 2 + g, :], Hb[:, h, :])
                if h % 2 == 1:
                    yield
            k.tt(y1[:].rr("p (h d) -> p h d", d=64), pyo[:, :].rr("p (h d) -> p h d", d=64),
                 eb[:].ub(2, 64), ALU.mult)
            k.tt(y1[:], y1[:], pyd[:, :], ALU.add)
            k.tt(y2[:].rr("p (h d) -> p h d", d=64), xs3, Dsk[:].ub(2, 64), ALU.mult)
            k.tt(y1[:], y1[:], y2[:], ALU.add)
            pdh = P[7].all()
            for h in range(8):
                g = h // 4
                k.mm(pdh[:, h * 64:(h + 1) * 64], BCb[:, g * 128:(g + 1) * 128], xdd[:, h, :])
            k.tt(Hf[:], Hf[:], ebl[:].ub(2, 64), ALU.mult)
            k.tt(Hf[:].rr("p h d -> p (h d)"), Hf[:].rr("p h d -> p (h d)"), pdh[:, :], ALU.add)
            k.copy(Hb[:], Hf[:], e="pool")
            yield
            act_sigmoid(sz[:], w3[:, 0:512])
            k.tt(sz[:], sz[:], w3[:, 0:512], ALU.mult, e="pool")
            k.tt(y1[:], y1[:], sz[:], ALU.mult)
            k.act(y2[:], y1[:], AF.Square)
            k.reduce(ssg[:], y2[:].rr("p (g d) -> p g d", d=256), ALU.add)
            act_rstd(ssg[:], 256)
            k.tt(y1[:].rr("p (g d) -> p g d", d=256), y1[:].rr("p (g d) -> p g d", d=256),
                 ssg[:].ub(2, 256), ALU.mult)
            k.tt(y2[:], y1[:], sng[:], ALU.mult)
            k.dma(MIX.u(ti)[r0:r0 + 128, 512:1024], y2[:], q="pool")
            yield

    def post_block(l, Xin, w_out, Xout):
        for s in range(2):
            with k.scope():
                h2T = k.sb("h2T", [128, 8, SEQ], BF16, nparts=NTS)
                acc = k.sb("acc", [128, NTS, 1024], nparts=NTS)
                comb = k.sb("comb", [128, NTS, 16], nparts=NTS)
                with k.scope():
                    woutb = k.sb("woutb", [128, 8, 1024], BF16)
                    stg = [k.sb(f"stgo{n}", [128, 1024]) for n in range(2)]
                    load_w_bf16(woutb, lambda kc: w_out[kc * 128:(kc + 1) * 128, :], 8, 1024, stg)
                    wrt = k.sb("wrt", [128, 8, 20])
                    for kc in range(8):
                        k.dma(wrt[:, kc, :], moe_w_rt.all()[l, kc * 128:(kc + 1) * 128, :])
                    n2g = crow(f"n2g{l}")
                    rb = crow(f"rb{l}")
                    mts = [k.sb(f"mt{n}", [128, 1024]) for n in range(2)]
                    xts = [k.sb(f"xr{n}", [128, 1024]) for n in range(2)]
                    mb_r = [k.sb(f"mb{n}", [128, 1024], BF16) for n in range(2)]
                    mT_r = [k.sb(f"mT{n}", [128, 8, 128], BF16) for n in range(2)]
                    junk = k.sb("junk2", [128, 1024])
                    hf_r = [k.sb(f"hf{n}", [128, 1024]) for n in range(2)]
                    hb16_r = [k.sb(f"hb16{n}", [128, 1024], BF16) for n in range(2)]
                    lo16_r = [k.sb(f"lo16{n}", [128, 1024], BF16) for n in range(2)]
                    loT_r = [k.sb(f"loT{n}", [128, 8, 128], BF16) for n in range(2)]
                    whi = k.sb("whi", [128, 8, 20], BF16)
                    wlo = k.sb("wlo", [128, 8, 20], BF16)
                    k.copy(whi[:], wrt[:])
                    k.tt(wlo[:], wrt[:], whi[:], ALU.subtract)
                    sm_r = [dict(rs=k.sb("rs2", [128, 1]), lg=k.sb("lg", [128, 20]), gmax=k.sb("gmax", [128, 1]),
                                 goh=k.sb("goh", [128, 4]), ge=k.sb("ge", [128, 4]), gsum=k.sb("gsum", [128, 1]),
                                 gpen=k.sb("gpen", [128, 4]), elm=k.sb("elm", [128, 16]), m8=k.sb("m8r", [128, 8]),
                                 sel=k.sb("sel", [128, 16]), ex=k.sb("ex", [128, 16]), dn=k.sb("dn", [128, 1]))
                            for n in range(2)]
                    for i in range(NTS if "D" in DBG else 0):
                        ti = s * NTS + i
                        r0 = ti * 128
                        mt, xt = mts[i % 2], xts[i % 2]
                        mb, mT, hf, hb16, lo16, loT = (mb_r[i % 2], mT_r[i % 2], hf_r[i % 2], hb16_r[i % 2],
                                                       lo16_r[i % 2], loT_r[i % 2])
                        sm = sm_r[i % 2]
                        rs, lg, gmax, goh, ge, gsum = sm["rs"], sm["lg"], sm["gmax"], sm["goh"], sm["ge"], sm["gsum"]
                        gpen, elm, m8, sel, ex, dn = sm["gpen"], sm["elm"], sm["m8"], sm["sel"], sm["ex"], sm["dn"]
                        k.dma(mt[:], MIX.u(ti)[r0:r0 + 128, :])
                        k.dma(xt[:], Xin.u(ti)[r0:r0 + 128, :])
                        k.copy(mb[:], mt[:], e="pool")
                        transpose_to(mT, mb, 8, [0, 1], ident_b)
                        for n in range(2):
                            pb = P[2 + n].all()
                            for kc in range(8):
                                k.mm(pb[:, :], mT[:, kc, :], woutb[:, kc, n * 512:(n + 1) * 512],
                                     start=(kc == 0), stop=(kc == 7))
                            k.tt(acc.u(i)[:, i, n * 512:(n + 1) * 512], pb[:, :], xt[:, n * 512:(n + 1) * 512], ALU.add)
                        x1 = acc.u(i)[:, i, :]
                        if "D1" not in DBG:
                            continue
                        rms_rstd(rs[:], x1, 1024, junk[:])
                        k.stt(hf[:], x1, rs[:], n2g[:], ALU.mult, ALU.mult)
                        k.copy(hb16[:], hf[:], e="act")
                        k.tt(lo16[:], hf[:], hb16[:], ALU.subtract)
                        for g in range(2):
                            pv = pbf(4 + g)
                            for q in range(4):
                                b = g * 4 + q
                                k.tr(pv[:, q * 128:(q + 1) * 128], hb16[:, b * 128:(b + 1) * 128], ident_b[:])
                            k.copy(h2T.u(i)[:, g * 4:(g + 1) * 4, i * 128:(i + 1) * 128],
                                   pv[:, 0:512].rr("p (a b) -> p a b", a=4), e=("act" if g else "dve"))
                        transpose_to(loT, lo16, 8, [6, 7], ident_b)
                        if "D2" not in DBG:
                            continue
                        pr = P[6].all()
                        nmm = 0
                        for kc in range(8):
                            for (a_, w_) in ((h2T.u(i)[:, kc, i * 128:(i + 1) * 128], whi), (loT[:, kc, :], whi),
                                             (h2T.u(i)[:, kc, i * 128:(i + 1) * 128], wlo)):
                                k.mm(pr[:, 0:20], a_, w_[:, kc, :], start=(nmm == 0), stop=(nmm == 23))
                                nmm += 1
                        k.tt(lg[:], pr[:, 0:20], rb[:], ALU.add)
                        k.reduce(gmax[:], lg[:, 0:4], ALU.max)
                        k.ts(goh[:], lg[:, 0:4], gmax[:], None, ALU.is_equal)
                        k.ts(gmax[:], gmax[:], -1.0, None, ALU.mult)
                        k.act(ge[:], lg[:, 0:4], AF.Exp, bias=gmax[:], accum_out=gsum[:])
                        k.recip(gsum[:], gsum[:])
                        k.ts(gpen[:], goh[:], 1.0, 30000.0, ALU.subtract, ALU.mult)
                        k.tt(elm[:].rr("p (g e) -> p g e", e=4), lg[:, 4:20].rr("p (g e) -> p g e", e=4),
                             gpen[:].ub(2, 4), ALU.add)
                        k.max8(m8[:], elm[:])
                        k.ts(sel[:], elm[:], m8[:, 1:2], None, ALU.is_ge)
                        k.ts(gmax[:], m8[:, 0:1], -1.0, None, ALU.mult)
                        k.act(ex[:], elm[:], AF.Exp, bias=gmax[:])
                        k.tt(ex[:], ex[:], sel[:], ALU.mult)
                        k.reduce(dn[:], ex[:], ALU.add)
                        k.recip(dn[:], dn[:])
                        k.tt(dn[:], dn[:], gsum[:], ALU.mult)
                        k.ts(comb.u(i)[:, i, :], ex[:], dn[:], None, ALU.mult)
                with k.scope():
                    sg_ = [k.sb(f"sg{n}", [128, 8, 256]) for n in range(2)]
                    sd_ = k.sb("sd", [128, 2, 1024])
                    wgb = [k.sb(f"wgb{n}", [128, 8, 256], BF16) for n in range(3)]
                    wub = [k.sb(f"wub{n}", [128, 8, 256], BF16) for n in range(3)]
                    wdb = [k.sb(f"wdb{n}", [128, 2, 1024], BF16) for n in range(3)]
                    sa = [k.sb(f"sa{n}", [128, 2, 512], BF16) for n in range(2)]
                    hid = [k.sb(f"hid{n}", [128, 2, 512], BF16) for n in range(2)]
                    def load_expert(e):
                        pe_ = e % 3
                        for kc in range(8):
                            k.dma(sg_[0][:, kc, :], moe_w_gate.all()[l, e, kc * 128:(kc + 1) * 128, :])
                        cast_rr(wgb[pe_][:], sg_[0][:])
                        for kc in range(8):
                            k.dma(sg_[1][:, kc, :], moe_w_up.all()[l, e, kc * 128:(kc + 1) * 128, :])
                        cast_rr(wub[pe_][:], sg_[1][:])
                        for fc in range(2):
                            k.dma(sd_[:, fc, :], moe_w_down.all()[l, e, fc * 128:(fc + 1) * 128, :])
                        cast_rr(wdb[pe_][:], sd_[:])

                    def gu_parts(idx, e, c):
                        pe_ = e % 3
                        sav, hv = sa[idx % 2], hid[idx % 2]
                        rhs_of = lambda kc: h2T.u(range(c * 4, c * 4 + 4))[:, kc, c * 512:(c + 1) * 512]

                        def gate(f):
                            pa = P[f].all()
                            for kc in range(8):
                                k.mm(pa[:, :], wgb[pe_][:, kc, f * 128:(f + 1) * 128], rhs_of(kc),
                                     start=(kc == 0), stop=(kc == 7))
                            k.act(sav[:, f, :], pa[:, :], AF.Silu)

                        def up(f):
                            pb = P[2 + f].all()
                            for kc in range(8):
                                k.mm(pb[:, :], wub[pe_][:, kc, f * 128:(f + 1) * 128], rhs_of(kc),
                                     start=(kc == 0), stop=(kc == 7))
                            k.tt(hv[:, f, :], pb[:, :], sav[:, f, :], ALU.mult)

                        return [lambda: gate(0), lambda: gate(1), lambda: up(0), lambda: up(1)]

                    def down_parts(idx, e, c):
                        pe_ = e % 3
                        hv = hid[idx % 2]

                        def one(t, n):
                            i = c * 4 + t
                            pd = P[4 + (2 * t + n) % 4].all()
                            for f in range(2):
                                k.mm(pd[:, :], hv[:, f, t * 128:(t + 1) * 128],
                                     wdb[pe_][:, f, n * 512:(n + 1) * 512],
                                     start=(f == 0), stop=(f == 1))
                            av = acc.u(i)[:, i, n * 512:(n + 1) * 512]
                            k.stt(av, pd[:, :], comb.u(i)[:, i, e:e + 1], av, ALU.mult, ALU.add)

                        return [(lambda t=t, n=n: one(t, n)) for t in range(4) for n in range(2)]

                    if "E" in DBG:
                        steps = [(e, c) for e in range(16) for c in range(4)]
                        load_expert(0)
                        load_expert(1)
                        for idx, (e, c) in enumerate(steps):
                            gp = gu_parts(idx, e, c)
                            dp = down_parts(idx - 1, *steps[idx - 1]) if idx > 0 else []
                            for u_ in range(4):
                                gp[u_]()
                                for d_ in dp[2 * u_:2 * u_ + 2]:
                                    d_()
                            if c == 0 and e + 2 < 16:
                                load_expert(e + 2)
                        for d_ in down_parts(len(steps) - 1, *steps[-1]):
                            d_()
                with k.scope():
                    wpg = k.sb("wpg", [128, 8, 1024], BF16)
                    wpp = k.sb("wpp", [128, 2, 1024], BF16)
                    stg = [k.sb(f"stgp{n}", [128, 1024]) for n in range(2)]
                    load_w_bf16(wpg, lambda kc: ple_w_gate.all()[l, kc * 128:(kc + 1) * 128, :], 8, 1024, stg)
                    load_w_bf16(wpp, lambda kc: ple_w_proj.all()[l, kc * 128:(kc + 1) * 128, :], 2, 1024, stg)
                    pgg = crow(f"pgg{l}")
                    pog = crow(f"pog{l}")
                    junk = k.sb("junk3", [128, 1024])
                    rs_r = [k.sb(f"rs3{n}", [128, 1]) for n in range(2)]
                    rsb_r = [k.sb(f"rs3b{n}", [128, 1]) for n in range(2)]
                    hb_r = [k.sb(f"hb3{n}", [128, 1024], BF16) for n in range(2)]
                    hT_r = [k.sb(f"hT3{n}", [128, 8, 128], BF16) for n in range(2)]
                    sgt_r = [k.sb(f"sgt{n}", [128, 1024]) for n in range(2)]
                    pts = [k.sb(f"pt{n}", [128, 256]) for n in range(2)]
                    ptb_r = [k.sb(f"ptb{n}", [128, 256], BF16) for n in range(2)]
                    pT_r = [k.sb(f"pT{n}", [128, 2, 128], BF16) for n in range(2)]
                    eg_r = [k.sb(f"eg{n}", [128, 1024]) for n in range(2)]
                    xo = [k.sb(f"xo{n}", [128, 1024]) for n in range(2)]
                    for i in range(NTS if "F" in DBG else 0):
                        ti = s * NTS + i
                        r0 = ti * 128
                        x2 = acc.u(i)[:, i, :]
                        pt_ = pts[i % 2]
                        rs, rsb, hb, hT, sgt, ptb, pT, eg = (rs_r[i % 2], rsb_r[i % 2], hb_r[i % 2], hT_r[i % 2],
                                                            sgt_r[i % 2], ptb_r[i % 2], pT_r[i % 2], eg_r[i % 2])
                        k.dma(pt_[:], p_in.all()[l, r0:r0 + 128, :])
                        rms_rstd(rs[:], x2, 1024, junk[:])
                        k.stt(hb[:], x2, rs[:], pgg[:], ALU.mult, ALU.mult)
                        transpose_to(hT, hb, 8, [0, 1], ident_b)
                        for n in range(2):
                            pb = P[2 + n].all()
                            for kc in range(8):
                                k.mm(pb[:, :], hT[:, kc, :], wpg[:, kc, n * 512:(n + 1) * 512],
                                     start=(kc == 0), stop=(kc == 7))
                            act_sigmoid(sgt[:, n * 512:(n + 1) * 512], pb[:, :])
                        k.copy(ptb[:], pt_[:], e="pool")
                        transpose_to(pT, ptb, 2, [4], ident_b)
                        for n in range(2):
                            pb = P[5 + n].all()
                            for kc in range(2):
                                k.mm(pb[:, :], pT[:, kc, :], wpp[:, kc, n * 512:(n + 1) * 512],
                                     start=(kc == 0), stop=(kc == 1))
                            k.tt(eg[:, n * 512:(n + 1) * 512], pb[:, :], sgt[:, n * 512:(n + 1) * 512], ALU.mult)
                        rms_rstd(rsb[:], eg[:], 1024, junk[:])
                        k.stt(eg[:], eg[:], rsb[:], pog[:], ALU.mult, ALU.mult)
                        o = xo[i % 2]
                        k.tt(o[:], eg[:], x2, ALU.add)
                        k.dma(Xout.u(ti)[r0:r0 + 128, :], o[:], q="pool")

    if stage in ("L0mix", "L0", "full"):
        layer0_mixers(x_in)
    if stage in ("L0", "full"):
        post_block(0, x_in, ev_w_out, XB)
    if stage == "L1mix":
        layer1_mixers(x_in)
    if stage == "L1":
        layer1_mixers(x_in)
        post_block(1, x_in, od_w_out, OUT)
    if stage == "POST":
        for ti in range(32):
            k.dma(MIX.u(ti)[ti * 128:(ti + 1) * 128, :], x_in.u(ti)[ti * 128:(ti + 1) * 128, :])
        post_block(0, x_in, ev_w_out, XB)
    if stage == "full":
        layer1_mixers(XB)
        post_block(1, XB, od_w_out, OUT)
    k.finish()
    return k


_BUILD_CACHE = {}


def prep_inputs(I):
    f = lambda a: np.ascontiguousarray(np.asarray(a, dtype=np.float32))
    shared = {
        "ev_w_in": f(I["ev_w_in"][0]),
        "ev_w_out": f(I["ev_w_out"][0]),
        "od_w_in": f(I["od_w_in"][0]),
        "od_w_out": f(I["od_w_out"][0]),
        "moe_w_rt": f(np.concatenate([np.asarray(I["moe_w_group"]), np.asarray(I["moe_w_router"])], axis=-1)),
        "moe_w_gate": f(np.asarray(I["moe_w_gate"]).reshape(2, 16, 1024, 256)),
        "moe_w_up": f(np.asarray(I["moe_w_up"]).reshape(2, 16, 1024, 256)),
        "moe_w_down": f(np.asarray(I["moe_w_down"]).reshape(2, 16, 256, 1024)),
        "ple_w_proj": f(I["ple_w_proj"]),
        "ple_w_gate": f(I["ple_w_gate"]),
        "cst": make_cst({kk: np.asarray(v) for kk, v in I.items()}),
        "blkind": (np.arange(SEQ)[None, :] // 256 == np.arange(8)[:, None]).astype(np.float32),
    }
    x = np.asarray(I["x"], dtype=np.float32).reshape(NCORES, TOK, 1024)
    p = np.asarray(I["p"], dtype=np.float32).reshape(2, NCORES, TOK, 256)
    maps = []
    for c in range(NCORES):
        m = dict(shared)
        m["x"] = np.ascontiguousarray(x[c])
        m["p"] = np.ascontiguousarray(p[:, c])
        maps.append(m)
    return maps


def run_stage(I, stage="full", outname="out", trace=False):
    if stage not in _BUILD_CACHE:
        _BUILD_CACHE[stage] = build(stage)
    kb = _BUILD_CACHE[stage]
    maps = prep_inputs(I)
    res = run_bass_kernel_spmd(kb.nc, maps, core_ids=list(range(NCORES)))
    return np.stack([np.asarray(r[outname]) for r in res.results], axis=0)


FUSED = True


def kernel(**inputs):
    if FUSED:
        o = run_stage(inputs, "full", "out")
    else:
        xb = run_stage(inputs, "L0", "xb")
        inputs2 = dict(inputs)
        inputs2["x"] = xb.reshape(16, SEQ, 1024)
        o = run_stage(inputs2, "L1", "out")
    return o.reshape(16, SEQ, 1024).astype(np.float32)
```
